# Optimizing a Trainium2 kernel written in Bass

```python
import math
import jax
import jax.numpy as jnp
from jax import lax
import numpy as np

D_MODEL = 1024
BATCH = 4
SEQ = 4096
DEPTH = 2

SB_HEADS = 4
SB_DIM = 64
DIFF_HEADS = 4
DIFF_DIM = 32
DIFF_V_DIM = 2 * DIFF_DIM
FOX_HEADS = 4
FOX_DIM = 64
MLA_HEADS = 4
MLA_Q_RANK = 256
MLA_KV_RANK = 128
MLA_NOPE_DIM = 64
MLA_ROPE_DIM = 32
MLA_V_DIM = 64
MIX_WIDTH = SB_HEADS * SB_DIM + DIFF_HEADS * DIFF_V_DIM + FOX_HEADS * FOX_DIM + MLA_HEADS * MLA_V_DIM
N_EXPERTS = 32
TOP_K = 4
D_FF = 1024
SWIGLU_LIMIT = 7.0
SWIGLU_ALPHA = 1.702
ROPE_THETA = 10000.0
Q_BLOCK = 128
MOE_BLOCK = 256
LN_EPS = 1e-5
RMS_EPS = 1e-6
DEEPNORM_ALPHA = (2 * DEPTH) ** 0.25
DEEPNORM_BETA = (8 * DEPTH) ** -0.25
IN_SPLITS = (SB_HEADS * SB_DIM, SB_HEADS * SB_DIM, SB_HEADS * SB_DIM,
             DIFF_HEADS * 2 * DIFF_DIM, DIFF_HEADS * 2 * DIFF_DIM, DIFF_HEADS * DIFF_V_DIM,
             FOX_HEADS * FOX_DIM, FOX_HEADS * FOX_DIM, FOX_HEADS * FOX_DIM, FOX_HEADS,
             MLA_Q_RANK, MLA_KV_RANK, MLA_ROPE_DIM)
IN_COLS = sum(IN_SPLITS)

kernel_name = 'hybrid_sb_diff_fox_mla_moe_deepnorm'


def _layer_norm(x, g, b):
    xf = x.astype(jnp.float32)
    mu = jnp.mean(xf, axis=-1, keepdims=True)
    var = jnp.mean(jnp.square(xf - mu), axis=-1, keepdims=True)
    y = (xf - mu) * lax.rsqrt(var + LN_EPS) * g.astype(jnp.float32) + b.astype(jnp.float32)
    return y.astype(x.dtype)


def _rms_norm(x, g):
    xf = x.astype(jnp.float32)
    y = xf * lax.rsqrt(jnp.mean(jnp.square(xf), axis=-1, keepdims=True) + RMS_EPS) * g.astype(jnp.float32)
    return y.astype(x.dtype)


def _rope(x, positions):
    d = x.shape[-1]
    inv_freq = ROPE_THETA ** (-jnp.arange(0, d, 2, dtype=jnp.float32) / d)
    ang = positions.astype(jnp.float32)[..., None] * inv_freq
    cos = jnp.cos(ang)[:, :, None, :]
    sin = jnp.sin(ang)[:, :, None, :]
    xf = x.astype(jnp.float32)
    x1, x2 = xf[..., : d // 2], xf[..., d // 2:]
    return jnp.concatenate([x1 * cos - x2 * sin, x2 * cos + x1 * sin], axis=-1).astype(x.dtype)


def _rows(t, q0):
    return lax.dynamic_slice_in_dim(t, q0, Q_BLOCK, axis=1)


def _causal_mask(q0, seq_len, strict):
    t = q0 + jnp.arange(Q_BLOCK)
    s = jnp.arange(seq_len)
    return (s[None, :] < t[:, None]) if strict else (s[None, :] <= t[:, None])


def _masked_softmax(logits, mask):
    return jax.nn.softmax(jnp.where(mask, logits, -jnp.inf), axis=-1)


def _sweep_query_blocks(block_fn, seq_len):
    starts = jnp.arange(seq_len // Q_BLOCK, dtype=jnp.int32) * Q_BLOCK
    out = lax.map(block_fn, starts)
    nb, b, qb, h, dv = out.shape
    return jnp.transpose(out, (1, 0, 2, 3, 4)).reshape(b, nb * qb, h, dv)


def _stick_breaking(q, k, v):
    seq_len = q.shape[1]
    scale = 1.0 / math.sqrt(q.shape[-1])

    def block(q0):
        z = jnp.einsum('bqhd,bkhd->bhqk', _rows(q, q0), k).astype(jnp.float32) * scale
        strict = _causal_mask(q0, seq_len, strict=True)
        log_keep = jnp.where(strict, jax.nn.log_sigmoid(-z), 0.0)
        after = lax.cumsum(log_keep, axis=3, reverse=True) - log_keep
        w = jnp.where(strict, jnp.exp(jax.nn.log_sigmoid(z) + after), 0.0)
        return jnp.einsum('bhqk,bkhd->bqhd', w.astype(v.dtype), v)

    return _sweep_query_blocks(block, seq_len)


def _differential(q1, q2, k1, k2, v, lam):
    seq_len = q1.shape[1]
    scale = 1.0 / math.sqrt(q1.shape[-1])

    def block(q0):
        mask = _causal_mask(q0, seq_len, strict=False)
        s1 = jnp.einsum('bqhd,bkhd->bhqk', _rows(q1, q0), k1).astype(jnp.float32) * scale
        s2 = jnp.einsum('bqhd,bkhd->bhqk', _rows(q2, q0), k2).astype(jnp.float32) * scale
        w = _masked_softmax(s1, mask) - lam * _masked_softmax(s2, mask)
        return jnp.einsum('bhqk,bkhd->bqhd', w.astype(v.dtype), v)

    return _sweep_query_blocks(block, seq_len)


def _forgetting(q, k, v, cum_log_f):
    seq_len = q.shape[1]
    scale = 1.0 / math.sqrt(q.shape[-1])
    f_key = jnp.transpose(cum_log_f, (0, 2, 1))[:, :, None, :]

    def block(q0):
        mask = _causal_mask(q0, seq_len, strict=False)
        f_q = jnp.transpose(_rows(cum_log_f, q0), (0, 2, 1))[:, :, :, None]
        s = jnp.einsum('bqhd,bkhd->bhqk', _rows(q, q0), k).astype(jnp.float32) * scale + f_q - f_key
        return jnp.einsum('bhqk,bkhd->bqhd', _masked_softmax(s, mask).astype(v.dtype), v)

    return _sweep_query_blocks(block, seq_len)


def _mla(q_nope, q_rope, k_nope, k_rope, v):
    seq_len = q_nope.shape[1]
    scale = 1.0 / math.sqrt(q_nope.shape[-1] + q_rope.shape[-1])

    def block(q0):
        mask = _causal_mask(q0, seq_len, strict=False)
        s = (jnp.einsum('bqhd,bkhd->bhqk', _rows(q_nope, q0), k_nope)
             + jnp.einsum('bqhr,bkr->bhqk', _rows(q_rope, q0), k_rope)).astype(jnp.float32) * scale
        return jnp.einsum('bhqk,bkhd->bqhd', _masked_softmax(s, mask).astype(v.dtype), v)

    return _sweep_query_blocks(block, seq_len)


def _hybrid_mixer(h, positions, lambda_init, w_in, b_forget, diff_lambda, diff_subln_g,
                  mla_q_norm_g, mla_w_uq, mla_kv_norm_g, mla_w_ukv, w_out):
    b, s, _ = h.shape
    split_points = np.cumsum(IN_SPLITS)[:-1].tolist()
    (sb_q, sb_k, sb_v, df_q, df_k, df_v, fx_q, fx_k, fx_v, fx_f,
     mla_cq, mla_ckv, mla_kr) = jnp.split(h @ w_in, split_points, axis=-1)

    def heads(t, n):
        return t.reshape(b, s, n, -1)

    out_a = _stick_breaking(heads(sb_q, SB_HEADS), heads(sb_k, SB_HEADS), heads(sb_v, SB_HEADS))

    dq = _rope(heads(df_q, 2 * DIFF_HEADS), positions).reshape(b, s, DIFF_HEADS, 2, DIFF_DIM)
    dk = _rope(heads(df_k, 2 * DIFF_HEADS), positions).reshape(b, s, DIFF_HEADS, 2, DIFF_DIM)
    lp = diff_lambda.astype(jnp.float32)
    lam = jnp.exp(jnp.sum(lp[0] * lp[1])) - jnp.exp(jnp.sum(lp[2] * lp[3])) + lambda_init
    out_b = _differential(dq[:, :, :, 0], dq[:, :, :, 1], dk[:, :, :, 0], dk[:, :, :, 1],
                          heads(df_v, DIFF_HEADS), lam)
    out_b = _rms_norm(out_b, diff_subln_g) * (1.0 - lambda_init)

    log_f = jax.nn.log_sigmoid((fx_f + b_forget).astype(jnp.float32))
    out_c = _forgetting(heads(fx_q, FOX_HEADS), heads(fx_k, FOX_HEADS), heads(fx_v, FOX_HEADS),
                        jnp.cumsum(log_f, axis=1))

    q_d = heads(_rms_norm(mla_cq, mla_q_norm_g) @ mla_w_uq, MLA_HEADS)
    q_nope, q_rope = q_d[..., :MLA_NOPE_DIM], _rope(q_d[..., MLA_NOPE_DIM:], positions)
    kv_d = heads(_rms_norm(mla_ckv, mla_kv_norm_g) @ mla_w_ukv, MLA_HEADS)
    k_nope, v_d = kv_d[..., :MLA_NOPE_DIM], kv_d[..., MLA_NOPE_DIM:]
    k_rope = _rope(mla_kr[:, :, None, :], positions)[:, :, 0, :]
    out_d = _mla(q_nope, q_rope, k_nope, k_rope, v_d)

    mixed = jnp.concatenate([out_a.reshape(b, s, -1), out_b.reshape(b, s, -1),
                             out_c.reshape(b, s, -1), out_d.reshape(b, s, -1)], axis=-1)
    return mixed @ w_out


def _clamped_swiglu(gu):
    glu, lin = gu[..., :D_FF], gu[..., D_FF:]
    glu = jnp.minimum(glu, SWIGLU_LIMIT)
    lin = jnp.clip(lin, -SWIGLU_LIMIT, SWIGLU_LIMIT)
    return glu * jax.nn.sigmoid(SWIGLU_ALPHA * glu) * (lin + 1.0)


def _moe(h, router_w, router_b, w_gate_up, b_gate_up, w_down, b_down):
    b, s, d = h.shape
    n_tok = b * s
    xt = h.reshape(n_tok, d)
    logits = (xt @ router_w + router_b).astype(jnp.float32)
    top_logit, top_exp = lax.top_k(logits, TOP_K)
    gate = jax.nn.softmax(top_logit, axis=-1)
    n_assign = n_tok * TOP_K
    flat_exp = top_exp.reshape(-1)
    flat_tok = jnp.arange(n_assign, dtype=jnp.int32) // TOP_K
    order = jnp.argsort(flat_exp)
    sorted_exp = flat_exp[order]
    sorted_tok = flat_tok[order]
    sorted_gate = gate.reshape(-1)[order]
    counts = jnp.bincount(flat_exp, length=N_EXPERTS)
    start = jnp.cumsum(counts) - counts
    padded = (counts + MOE_BLOCK - 1) // MOE_BLOCK * MOE_BLOCK
    padded_end = jnp.cumsum(padded)
    padded_start = padded_end - padded
    slot = padded_start[sorted_exp] + (jnp.arange(n_assign, dtype=jnp.int32) - start[sorted_exp])
    n_blocks = n_assign // MOE_BLOCK + N_EXPERTS
    n_slots = n_blocks * MOE_BLOCK
    slot_tok = jnp.full((n_slots,), n_tok, jnp.int32).at[slot].set(sorted_tok)
    x_pad = jnp.concatenate([xt, jnp.zeros((1, d), xt.dtype)], axis=0)
    x_slots = x_pad[slot_tok].reshape(n_blocks, MOE_BLOCK, d)
    block_start = jnp.arange(n_blocks, dtype=jnp.int32) * MOE_BLOCK
    block_exp = jnp.minimum(jnp.sum(padded_end[None, :] <= block_start[:, None], axis=1), N_EXPERTS - 1)

    def expert_block(args):
        xb, e = args
        gu = xb @ w_gate_up[e] + b_gate_up[e]
        return _clamped_swiglu(gu) @ w_down[e] + b_down[e]

    y_slots = lax.map(expert_block, (x_slots, block_exp)).reshape(n_slots, d)
    y_assign = y_slots[slot] * sorted_gate[:, None].astype(h.dtype)
    y = jax.ops.segment_sum(y_assign, sorted_tok, num_segments=n_tok)
    return y.reshape(b, s, d)


def setup_inputs(seed: int = 0) -> dict:
    key = jax.random.key(seed)
    ks = jax.random.split(key, 24)
    f32 = jnp.float32

    def nrm(k, shape, scale):
        return jax.random.normal(k, shape, f32) * scale

    def gain(k, shape):
        return 1.0 + 0.02 * jax.random.normal(k, shape, f32)

    return {
        'x': nrm(ks[0], (BATCH, SEQ, D_MODEL), 1.0),
        'positions': jnp.broadcast_to(jnp.arange(SEQ, dtype=jnp.int32), (BATCH, SEQ)),
        'ln_in_g': gain(ks[1], (D_MODEL,)),
        'ln_in_b': nrm(ks[2], (D_MODEL,), 0.02),
        'w_in': nrm(ks[3], (DEPTH, D_MODEL, IN_COLS), D_MODEL ** -0.5),
        'b_forget': 3.0 + nrm(ks[4], (DEPTH, FOX_HEADS), 0.1),
        'diff_lambda': nrm(ks[5], (DEPTH, 4, DIFF_DIM), 0.1),
        'diff_subln_g': gain(ks[6], (DEPTH, DIFF_V_DIM)),
        'mla_q_norm_g': gain(ks[7], (DEPTH, MLA_Q_RANK)),
        'mla_w_uq': nrm(ks[8], (DEPTH, MLA_Q_RANK, MLA_HEADS * (MLA_NOPE_DIM + MLA_ROPE_DIM)), MLA_Q_RANK ** -0.5),
        'mla_kv_norm_g': gain(ks[9], (DEPTH, MLA_KV_RANK)),
        'mla_w_ukv': nrm(ks[10], (DEPTH, MLA_KV_RANK, MLA_HEADS * (MLA_NOPE_DIM + MLA_V_DIM)), MLA_KV_RANK ** -0.5),
        'w_out': nrm(ks[11], (DEPTH, MIX_WIDTH, D_MODEL), MIX_WIDTH ** -0.5 * DEEPNORM_BETA),
        'ln1_g': gain(ks[12], (DEPTH, D_MODEL)),
        'ln1_b': nrm(ks[13], (DEPTH, D_MODEL), 0.02),
        'router_w': nrm(ks[14], (DEPTH, D_MODEL, N_EXPERTS), D_MODEL ** -0.5),
        'router_b': nrm(ks[15], (DEPTH, N_EXPERTS), 0.01),
        'w_gate_up': nrm(ks[16], (DEPTH, N_EXPERTS, D_MODEL, 2 * D_FF), D_MODEL ** -0.5),
        'b_gate_up': nrm(ks[17], (DEPTH, N_EXPERTS, 2 * D_FF), 0.01),
        'w_down': nrm(ks[18], (DEPTH, N_EXPERTS, D_FF, D_MODEL), D_FF ** -0.5 * DEEPNORM_BETA),
        'b_down': nrm(ks[19], (DEPTH, N_EXPERTS, D_MODEL), 0.01),
        'ln2_g': gain(ks[20], (DEPTH, D_MODEL)),
        'ln2_b': nrm(ks[21], (DEPTH, D_MODEL), 0.02),
    }


def reference(x, positions, ln_in_g, ln_in_b, w_in, b_forget, diff_lambda, diff_subln_g,
              mla_q_norm_g, mla_w_uq, mla_kv_norm_g, mla_w_ukv, w_out, ln1_g, ln1_b,
              router_w, router_b, w_gate_up, b_gate_up, w_down, b_down, ln2_g, ln2_b):
    h = _layer_norm(x, ln_in_g, ln_in_b)
    for l in range(DEPTH):
        lambda_init = 0.8 - 0.6 * math.exp(-0.3 * l)
        mix = _hybrid_mixer(h, positions, lambda_init, w_in[l], b_forget[l], diff_lambda[l],
                            diff_subln_g[l], mla_q_norm_g[l], mla_w_uq[l], mla_kv_norm_g[l],
                            mla_w_ukv[l], w_out[l])
        h = _layer_norm(DEEPNORM_ALPHA * h + mix, ln1_g[l], ln1_b[l])
        ffn = _moe(h, router_w[l], router_b[l], w_gate_up[l], b_gate_up[l], w_down[l], b_down[l])
        h = _layer_norm(DEEPNORM_ALPHA * h + ffn, ln2_g[l], ln2_b[l])
    return h
```

```python
import math
import os
from contextlib import ExitStack

import numpy as np
import ml_dtypes
import concourse.bass as bass
import concourse.mybir as mybir
from concourse.bass_utils import run_bass_kernel_spmd

F32 = mybir.dt.float32
BF16 = mybir.dt.bfloat16
I32 = mybir.dt.int32
AF = mybir.ActivationFunctionType
ALU = mybir.AluOpType
AX = mybir.AxisListType

D = 1024
DEPTH = 2
NE = 32
TOPK = 4
DFF = 1024
LN_EPS = 1e-5
RMS_EPS = 1e-6
ALPHA = (2 * DEPTH) ** 0.25
ROPE_THETA = 10000.0
NWIN = 768 + 1792 + 772 + 640


class Prog:
    ENG = ("pe", "act", "dve", "pool", "sp")

    def __init__(self, nc, es, n_dma_sems=24):
        self.nc = nc
        self._es = es
        self.lists = {e: [] for e in self.ENG}
        self.sem = {e: es.enter_context(nc.semaphore("s_" + e)) for e in self.ENG}
        self.cnt = {e: 0 for e in self.ENG}
        self.dsems = [es.enter_context(nc.semaphore(f"d{i}")) for i in range(n_dma_sems)]
        self.dcnt = [0] * n_dma_sems
        a, b = n_dma_sems // 2, n_dma_sems // 2 + n_dma_sems // 6
        self.dpool = {"sp": list(range(0, a)), "act": list(range(a, b)), "pool": list(range(b, n_dma_sems))}
        self.dnext = {"sp": 0, "act": 0, "pool": 0}
        self.waited = {}
        self.lastw = {}
        self.readers = {}

    def _semh(self, key):
        return self.sem[key[1]] if key[0] == "e" else self.dsems[key[1]]

    def _deps(self, eng, reads, writes, extra=()):
        need = {}

        def add(tok):
            if tok is None:
                return
            k, v = tok
            if need.get(k, 0) < v:
                need[k] = v

        for r in reads:
            add(self.lastw.get(r))
        for w in writes:
            add(self.lastw.get(w))
            for k, v in self.readers.get(w, {}).items():
                add((k, v))
        for t in extra:
            add(t)
        waits = []
        for k, v in need.items():
            if eng == "pe" and k == ("e", "pe"):
                continue
            if self.waited.get((eng, k), 0) >= v:
                continue
            self.waited[(eng, k)] = v
            waits.append((self._semh(k), v))
        return waits

    def _record(self, tok, reads, writes):
        for w in writes:
            self.lastw[w] = tok
            self.readers[w] = {}
        for r in reads:
            d = self.readers.setdefault(r, {})
            if d.get(tok[0], 0) < tok[1]:
                d[tok[0]] = tok[1]

    def op(self, eng, fn, reads=(), writes=()):
        waits = self._deps(eng, reads, writes)
        self.cnt[eng] += 1
        tok = (("e", eng), self.cnt[eng])
        self.lists[eng].append((waits, fn, (self.sem[eng], 1)))
        self._record(tok, reads, writes)
        return tok

    def dma(self, q, fn, reads=(), writes=()):
        pl = self.dpool[q]
        i = pl[self.dnext[q]]
        self.dnext[q] = (self.dnext[q] + 1) % len(pl)
        prev = (("d", i), self.dcnt[i]) if self.dcnt[i] else None
        waits = self._deps(q, reads, writes, extra=(prev,) if prev else ())
        self.dcnt[i] += 16
        tok = (("d", i), self.dcnt[i])
        self.lists[q].append((waits, fn, (self.dsems[i], 16)))
        self._record(tok, reads, writes)
        return tok

    def cc(self, fn, reads=(), writes=(), inc=1):
        if not hasattr(self, "ccsem"):
            self.ccsem = self._es.enter_context(self.nc.semaphore("ccsem"))
            self.dsems.append(self.ccsem)
            self.dcnt.append(0)
        i = len(self.dsems) - 1
        prev = (("d", i), self.dcnt[i]) if self.dcnt[i] else None
        waits = self._deps("pool", reads, writes, extra=(prev,) if prev else ())
        self.dcnt[i] += inc
        tok = (("d", i), self.dcnt[i])
        self.lists["pool"].append((waits, fn, (self.dsems[i], inc)))
        self._record(tok, reads, writes)
        return tok

    def finish(self, eng="sp"):
        waits = []
        for i, c in enumerate(self.dcnt):
            if c and self.waited.get((eng, ("d", i)), 0) < c:
                waits.append((self.dsems[i], c))
        for e in self.ENG:
            if e != eng and self.cnt[e] and self.waited.get((eng, ("e", e)), 0) < self.cnt[e]:
                waits.append((self.sem[e], self.cnt[e]))
        self.lists[eng].append((waits, None, None))

    def barrier(self):
        for eng in self.ENG:
            waits = []
            for i, c in enumerate(self.dcnt):
                if c and self.waited.get((eng, ("d", i)), 0) < c:
                    waits.append((self.dsems[i], c))
                    self.waited[(eng, ("d", i))] = c
            for e in self.ENG:
                if self.cnt[e] and self.waited.get((eng, ("e", e)), 0) < self.cnt[e]:
                    waits.append((self.sem[e], self.cnt[e]))
                    self.waited[(eng, ("e", e))] = self.cnt[e]
            self.lists[eng].append((waits, None, None))

    def emit(self):
        nc = self.nc
        lists = self.lists

        def run(engobj, items):
            for waits, fn, inc in items:
                for s, v in waits:
                    engobj.wait_ge(s, v)
                if fn is None:
                    continue
                ins = fn(engobj)
                if inc is not None:
                    ins.then_inc(inc[0], inc[1])

        with nc.Block() as block:
            @block.tensor
            def _(e):
                run(e, lists["pe"])

            @block.scalar
            def _(e):
                run(e, lists["act"])

            @block.vector
            def _(e):
                run(e, lists["dve"])

            @block.gpsimd
            def _(e):
                run(e, lists["pool"])

            @block.sync
            def _(e):
                run(e, lists["sp"])


class Rot:
    def __init__(self, items):
        self.items = items
        self.i = 0

    def next(self):
        it = self.items[self.i]
        self.i = (self.i + 1) % len(self.items)
        return it


def host_consts():
    k = np.arange(128)[:, None]
    q = np.arange(128)[None, :]
    c = {}
    c["ident"] = np.eye(128, dtype=np.float32)
    c["maskd"] = (q >= k).astype(np.float32)
    c["masks"] = (q > k).astype(np.float32)
    c["negtri"] = np.where(k >= q, -8.0, 0.0).astype(np.float32)
    c["triincl"] = (k <= q).astype(np.float32)
    c["ones"] = np.ones((128, 128), np.float32)
    e0 = np.zeros((128, 128), np.float32)
    e0[0, :] = 1.0
    c["e0"] = e0
    p = np.arange(128)
    i = p % 16
    freq = (ROPE_THETA ** (-(2.0 * i) / 32.0)) / (2.0 * math.pi)
    freq = np.where(p < 96, freq, 0.0)
    sgn = np.where((p % 32) < 16, -1.0, 1.0)
    c["ropef"] = np.stack([freq, sgn], axis=1).astype(np.float32)
    c["iota_e"] = np.tile(np.arange(NE, dtype=np.float32)[None, :], (128, 1))
    c["tristrict"] = (k < q).astype(np.float32)
    return c


def prep_w_in(w, hp=(0, 1, 2, 3)):
    out = np.zeros((D, NWIN), np.float32)
    o = 0
    for base in (0, 256, 512):
        for h in range(4):
            out[:, o + h * 64:o + h * 64 + 64] = w[:, base + hp[h] * 64:base + hp[h] * 64 + 64]
        o += 256
    for src in (768, 1024):
        for rot in (0, 1):
            for u in range(8):
                ug = 2 * hp[u // 2] + (u % 2)
                dst = o + (u // 3) * 128 + (u % 3) * 32
                s0 = src + ug * 32
                if rot == 0:
                    out[:, dst:dst + 32] = w[:, s0:s0 + 32]
                else:
                    out[:, dst:dst + 16] = w[:, s0 + 16:s0 + 32]
                    out[:, dst + 16:dst + 32] = w[:, s0:s0 + 16]
            o += 384
    for h in range(4):
        out[:, o + h * 64:o + h * 64 + 64] = w[:, 1280 + hp[h] * 64:1280 + hp[h] * 64 + 64]
    o += 256
    for base in (1536, 1792, 2048):
        for h in range(4):
            out[:, o + h * 64:o + h * 64 + 64] = w[:, base + hp[h] * 64:base + hp[h] * 64 + 64]
        o += 256
    for h in range(4):
        out[:, o + h] = w[:, 2304 + hp[h]]
    o += 4
    out[:, o:o + 384] = w[:, 2308:2692]
    o += 384
    out[:, o + 64:o + 96] = w[:, 2692:2724]
    o += 128
    out[:, o + 64:o + 80] = w[:, 2692 + 16:2724]
    out[:, o + 80:o + 96] = w[:, 2692:2692 + 16]
    o += 128
    assert o == NWIN
    return out


def prep_w_uq(w, hp=(0, 1, 2, 3)):
    out = np.zeros((256, 1024), np.float32)
    for h in range(4):
        g = hp[h]
        a = h * 256
        out[:, a:a + 96] = w[:, g * 96:g * 96 + 96]
        b = a + 128
        out[:, b + 64:b + 80] = w[:, g * 96 + 80:g * 96 + 96]
        out[:, b + 80:b + 96] = w[:, g * 96 + 64:g * 96 + 80]
    return out


def prep_w_ukv(w, hp=(0, 1, 2, 3)):
    out = np.zeros((128, 768), np.float32)
    for h in range(4):
        g = hp[h]
        out[:, h * 128:h * 128 + 64] = w[:, g * 128:g * 128 + 64]
        out[:, 512 + h * 64:512 + h * 64 + 64] = w[:, g * 128 + 64:g * 128 + 128]
    return out


def phase_a(nc, P, es, S, T, pre_ln, lam_init, tag, stop_after=9, NHP=4):
    NT = S // 128
    cur = [es]
    NJ = S // 512

    def sb(name, shape, dt):
        return cur[0].enter_context(nc.sbuf_tensor(tag + name, shape, dt))

    def ps(name, shape, dt):
        return es.enter_context(nc.psum_tensor(tag + name, shape, dt))

    hT = sb("hT", [128, 8, S], BF16)
    identb = sb("identb", [128, 128], BF16)
    _ob = [(ps(f"O{i}", [128, 512], F32), f"O{i}") for i in range(4)]
    Ob = Rot(_ob)
    ObSB = Rot(_ob[0:3])
    ObA = Rot(_ob[0:2])
    Pb = Rot(_ob[2:4])
    pools = {"S": None, "O": None}
    Tb = ps("Tb", [128, 8, 128], BF16)

    def ld(q, dst, src, key):
        P.dma(q, lambda e: e.dma_start(out=dst, in_=src), writes=[key])

    ld("pool", identb[:], T["ident"], "identb")
    es_h = ExitStack()
    cur[0] = es_h
    hld = Rot([(sb(f"hld{i}", [128, 1024], F32), f"hld{i}") for i in range(2)])
    hbf = Rot([(sb(f"hbf{i}", [128, 1024], BF16), f"hbf{i}") for i in range(2)])
    st6 = sb("st6", [128, 2, 6], F32)
    mv = sb("mv", [128, 2], F32)
    if pre_ln:
        lngb = sb("lngb", [128, 1024], F32)
        lnbb = sb("lnbb", [128, 1024], F32)
    if pre_ln:
        ld("sp", lngb[:], T["lng"].partition_broadcast(128), "lngb")
        ld("sp", lnbb[:], T["lnb"].partition_broadcast(128), "lnbb")
    def layer_norm_tile(src, srckey, dst, dstkey, gb, bb, gk, bk_):
        for c in range(2):
            P.op("dve", lambda e, c=c: e.bn_stats(out=st6[:, c, :], in_=src[:, c * 512:(c + 1) * 512]),
                 reads=[srckey], writes=["st6"])
        P.op("dve", lambda e: e.bn_aggr(out=mv[:], in_=st6[:].rearrange("p a b -> p (a b)")), reads=["st6"],
             writes=["mv"])
        P.op("act", lambda e: e.activation(out=mv[:, 1:2], in_=mv[:, 1:2], func=AF.Ln, bias=float(LN_EPS)),
             reads=["mv"], writes=["mv"])
        P.op("act", lambda e: e.activation(out=mv[:, 1:2], in_=mv[:, 1:2], func=AF.Exp, scale=-0.5),
             reads=["mv"], writes=["mv"])
        P.op("dve", lambda e: e.scalar_tensor_tensor(out=dst, in0=src, scalar=mv[:, 0:1], in1=gb, op0=ALU.subtract,
                                                     op1=ALU.mult), reads=[srckey, "mv", gk], writes=[dstkey])
        P.op("dve", lambda e: e.scalar_tensor_tensor(out=dst, in0=dst, scalar=mv[:, 1:2], in1=bb, op0=ALU.mult,
                                                     op1=ALU.add), reads=[dstkey, "mv", bk_], writes=[dstkey])

    for t in range(NT):
        ht, hk = hld.next()
        hb, hbk = hbf.next()
        src_t = T["hin_tile"](t) if "hin_tile" in T else T["hin"][t * 128:(t + 1) * 128, :]
        P.dma("sp", lambda e, ht=ht, src_t=src_t: e.dma_start(out=ht[:], in_=src_t), writes=[hk])
        if pre_ln:
            layer_norm_tile(ht[:], hk, ht[:], hk, lngb[:], lnbb[:], "lngb", "lnbb")
            P.dma("act", lambda e, ht=ht, t=t: e.dma_start(out=T["h0"][t * 128:(t + 1) * 128, :], in_=ht[:]), reads=[hk])
        P.op("act", lambda e, ht=ht, hb=hb: e.copy(out=hb[:], in_=ht[:]), reads=[hk], writes=[hbk])
        for c in range(8):
            P.op("pe", lambda e, hb=hb, c=c: e.transpose(out=Tb[:, c, :], in_=hb[:, c * 128:(c + 1) * 128],
                                                          identity=identb[:]), reads=[hbk, "identb"], writes=["Tb"])
        P.op("dve", lambda e, t=t: e.tensor_copy(out=hT[:, :, t * 128:(t + 1) * 128], in_=Tb[:]), reads=["Tb"],
             writes=[("hT", t // 4)])

    P.barrier()
    es_h.close()
    cur[0] = es
    KT = sb("KT", [128, 4, S], BF16)
    V = sb("V", [128, NT, 4, 65], BF16)
    QT = [sb(f"QT{i}", [128, 4, 512], BF16) for i in range(2)]
    Wm = sb("Wm", [128, 8, 1792], BF16)
    Wuq = sb("Wuq", [128, 2, 1024], BF16)
    Wukv = sb("Wukv", [128, 768], BF16)
    mD = sb("mD", [128, 128], BF16)
    mS = sb("mS", [128, 128], BF16)
    negtri = sb("negtri", [128, 128], BF16)
    onesb = sb("onesb", [128, 128], BF16)
    trib = sb("trib", [128, 128], BF16)
    e0b = sb("e0b", [128, 128], BF16)
    lsp = sb("lsp", [128, 4, 3, 4], BF16)
    lr1 = sb("lr1", [128, 4, 4], F32)
    lr2 = sb("lr2", [128, 4, 4], F32)
    ropef = sb("ropef", [128, 2], F32)
    cosT = sb("cosT", [128, 512], F32)
    sinT = sb("sinT", [128, 512], F32)
    posi = sb("posi", [128, 512], I32)
    posf = sb("posf", [128, 512], F32)
    rt1 = sb("rt1", [128, 512], F32)
    rt2 = sb("rt2", [128, 512], F32)
    rti = sb("rti", [128, 512], I32)
    PTs = Rot([(sb(f"PT{i}", [128, 512], BF16), f"PT{i}") for i in range(3)])
    E32 = Rot([(sb(f"E32{i}", [128, 512], F32), f"E32{i}") for i in range(2)])
    SPB = Rot([(sb(f"SPB{i}", [128, 512], BF16), f"SPB{i}") for i in range(4)])
    osb = sb("osb", [128, 4, 64], F32)
    tmpo = sb("tmpo", [128, 4, 64], F32)
    tmpo2 = sb("tmpo2", [128, 4, 64], F32)
    fbuf = sb("fbuf", [128, 4], F32)
    rinv = sb("rinv", [128, 8], F32)
    stage = Rot([(sb(f"stage{i}", [128, 4, 256], BF16), f"stage{i}") for i in range(2)])
    bfb = sb("bfb", [128, 4], F32)
    dlb = sb("dlb", [128, 128], F32)
    dgb = sb("dgb", [128, 4, 64], F32)
    qgb = sb("qgb", [128, 256], F32)
    kvgb = sb("kvgb", [128, 128], F32)
    lam = sb("lam", [128, 4], F32)
    xg = sb("xg", [128, 4, 4], F32)
    lgt = sb("lgt", [128, 4, 4], F32)
    G = sb("G", [128, NT, 4], F32)
    Gc = sb("Gc", [128, 4], F32)
    Gmid = sb("Gmid", [128, 4], F32)
    FB = sb("FB", [128, 2, 4, NT], F32)
    cqs = sb("cqs", [128, 4, 384], BF16)
    cqT = sb("cqT", [128, 3, 512], BF16)
    krope = sb("krope", [128, 512], BF16)
    qtmp = sb("qtmp", [128, 512], BF16)
    ssq = sb("ssq", [128, 8], F32)
    junk = sb("junk", [128, 256], F32)

    Sb = Rot([(ps(f"S{i}", [128, 512], F32), f"S{i}") for i in range(3)])
    pools["S"], pools["O"] = Sb, Ob
    ld("pool", mD[:], T["maskd"], "mD")
    ld("pool", mS[:], T["masks"], "mS")
    ld("pool", negtri[:], T["negtri"], "negtri")
    ld("pool", onesb[:], T["ones"], "onesb")
    ld("pool", trib[:], T["triincl"], "trib")
    ld("pool", e0b[:], T["e0"], "e0b")
    ld("sp", ropef[:], T["ropef"], "ropef")
    ld("sp", bfb[:], T["bf"].partition_broadcast(128), "bfb")
    ld("sp", dlb[:], T["dlam"].partition_broadcast(128), "dlb")
    for q_ in range(4):
        ld("sp", dgb[:, q_, :], T["dg"].partition_broadcast(128), "dgb")
    ld("sp", qgb[:], T["qg"].partition_broadcast(128), "qgb")
    ld("sp", kvgb[:], T["kvg"].partition_broadcast(128), "kvgb")
    ld("pool", Wuq[:], T["wuq"].rearrange("(c p) n -> p c n", p=128), "Wuq")
    ld("pool", Wukv[:], T["wukv"], "Wukv")
    P.op("pool", lambda e: e.memset(V[:], 1.0), writes=["Vall"])
    P.op("dve", lambda e: e.tensor_tensor(out=junk[:, 0:32], in0=dlb[:, 0:32], in1=dlb[:, 32:64], op=ALU.mult),
         reads=["dlb"], writes=["junk"])
    P.op("dve", lambda e: e.reduce_sum(out=lam[:, 1:2], in_=junk[:, 0:32], axis=AX.X), reads=["junk"], writes=["lam1"])
    P.op("dve", lambda e: e.tensor_tensor(out=junk[:, 32:64], in0=dlb[:, 64:96], in1=dlb[:, 96:128], op=ALU.mult),
         reads=["dlb"], writes=["junk"])
    P.op("dve", lambda e: e.reduce_sum(out=lam[:, 2:3], in_=junk[:, 32:64], axis=AX.X), reads=["junk"], writes=["lam2"])
    P.op("act", lambda e: e.activation(out=lam[:, 1:3], in_=lam[:, 1:3], func=AF.Exp), reads=["lam1", "lam2"],
         writes=["lam1", "lam2"])
    P.op("dve", lambda e: e.scalar_tensor_tensor(out=lam[:, 0:1], in0=lam[:, 2:3], scalar=-float(lam_init),
                                                 in1=lam[:, 1:2], op0=ALU.add, op1=ALU.subtract),
         reads=["lam1", "lam2"], writes=["lam0"])
    P.op("dve", lambda e: e.tensor_scalar(out=dgb[:], in0=dgb[:], scalar1=float(1.0 - lam_init), scalar2=None,
                                          op0=ALU.mult), reads=["dgb"], writes=["dgb"])

    def proj_fm(j, wcol, dst_ap, dstkey, post=None):
        bank, bk = pools["S"].next()
        for kc in range(8):
            P.op("pe", lambda e, kc=kc, bank=bank: e.matmul(bank[:], lhsT=Wm[:, kc, wcol:wcol + 128],
                                                             rhs=hT[:, kc, j * 512:(j + 1) * 512],
                                                             start=(kc == 0), stop=(kc == 7)),
                 reads=["Wm", ("hT", j)], writes=[bk])
        if post is None:
            P.op("dve", lambda e, bank=bank: e.tensor_copy(out=dst_ap, in_=bank[:]), reads=[bk], writes=[dstkey])
        return bank, bk

    def rope_tables(j):
        P.dma("sp", lambda e: e.dma_start(out=posi[:], in_=T["pos"][j * 512:(j + 1) * 512].partition_broadcast(128)),
              writes=["posi"])
        P.op("dve", lambda e: e.tensor_copy(out=posf[:], in_=posi[:]), reads=["posi"], writes=["posf"])
        for tab, off, key in ((sinT, 0.0, "sinT"), (cosT, 0.25, "cosT")):
            P.op("dve", lambda e, off=off: e.tensor_scalar(out=rt1[:], in0=posf[:], scalar1=ropef[:, 0:1], scalar2=off,
                                                           op0=ALU.mult, op1=ALU.add), reads=["posf", "ropef"],
                 writes=["rt1"])
            P.op("dve", lambda e: e.tensor_copy(out=rti[:], in_=rt1[:]), reads=["rt1"], writes=["rti"])
            P.op("dve", lambda e: e.tensor_copy(out=rt2[:], in_=rti[:]), reads=["rti"], writes=["rt2"])
            P.op("dve", lambda e: e.tensor_tensor(out=rt1[:], in0=rt1[:], in1=rt2[:], op=ALU.subtract),
                 reads=["rt1", "rt2"], writes=["rt1"])
            P.op("dve", lambda e: e.tensor_scalar(out=rt2[:], in0=rt1[:], scalar1=0.5, scalar2=None, op0=ALU.is_gt),
                 reads=["rt1"], writes=["rt2"])
            P.op("dve", lambda e: e.tensor_tensor(out=rt1[:], in0=rt1[:], in1=rt2[:], op=ALU.subtract),
                 reads=["rt1", "rt2"], writes=["rt1"])
            P.op("dve", lambda e: e.tensor_scalar(out=rt2[:], in0=rt1[:], scalar1=-0.5, scalar2=None, op0=ALU.is_lt),
                 reads=["rt1"], writes=["rt2"])
            P.op("dve", lambda e: e.tensor_tensor(out=rt1[:], in0=rt1[:], in1=rt2[:], op=ALU.add),
                 reads=["rt1", "rt2"], writes=["rt1"])
            P.op("act", lambda e, tab=tab: e.activation(out=tab[:], in_=rt1[:], func=AF.Sin, scale=2.0 * math.pi),
                 reads=["rt1"], writes=[key])
        P.op("dve", lambda e: e.tensor_scalar(out=sinT[:], in0=sinT[:], scalar1=ropef[:, 1:2], scalar2=None,
                                              op0=ALU.mult), reads=["sinT", "ropef"], writes=["sinT"])

    def rope_evac(bA, kA, bB, kB, dst_ap, dstkey, p0, p1):
        P.op("dve", lambda e: e.tensor_tensor(out=rt1[p0:p1, :], in0=bA[p0:p1, :], in1=cosT[p0:p1, :], op=ALU.mult),
             reads=[kA, "cosT"], writes=["rt1"])
        P.op("dve", lambda e: e.tensor_tensor(out=rt2[p0:p1, :], in0=bB[p0:p1, :], in1=sinT[p0:p1, :], op=ALU.mult),
             reads=[kB, "sinT"], writes=["rt2"])
        P.op("pool", lambda e: e.tensor_tensor(out=dst_ap, in0=rt1[p0:p1, :], in1=rt2[p0:p1, :], op=ALU.add),
             reads=["rt1", "rt2"], writes=[dstkey])

    def zero_qt():
        for i_ in range(2):
            P.op("dve", lambda e, i_=i_: e.memset(QT[i_][:], 0.0), writes=[("QT", i_)])

    def proj_q_padded(j, wcol, qt, qkey, c):
        bank, bk = proj_fm(j, wcol, None, None, post=True)
        for hh in range(2):
            P.op("dve", lambda e, bank=bank, hh=hh: e.tensor_copy(out=qt[64 * hh:64 * hh + 64, 2 * c + hh, :],
                                                                  in_=bank[64 * hh:64 * hh + 64, :]),
                 reads=[bk], writes=[qkey])

    def proj_v(j, wcol, ncols, extra=None):
        for t in range(4):
            tt = 4 * j + t
            bank, bk = pools["O"].next()
            for kc in range(8):
                P.op("pe", lambda e, kc=kc, bank=bank, tt=tt: e.matmul(bank[:, 0:ncols],
                                                                       lhsT=hT[:, kc, tt * 128:(tt + 1) * 128],
                                                                       rhs=Wm[:, kc, wcol:wcol + ncols],
                                                                       start=(kc == 0), stop=(kc == 7)),
                     reads=["Wm", ("hT", j)], writes=[bk])
            P.op("dve", lambda e, bank=bank, tt=tt: e.tensor_copy(out=V[:, tt, :, 0:64],
                                                                  in_=bank[:, 0:256].rearrange("p (a b) -> p a b", b=64)),
                 reads=[bk, "Vall"], writes=[("V", j)])
            if extra is not None:
                extra(t, bank, bk)

    def pv_and_norm(h, j, ob, obk):
        pass

    tile_hook = [None]

    def run_pipelined(pre, att):
        def step(gen):
            pools["S"], pools["O"] = Pb, Pb
            try:
                return next(gen, "done")
            finally:
                pools["S"], pools["O"] = Sb, Ob

        g0 = pre(0)
        while step(g0) != "done":
            pass
        for j in range(NJ):
            nxt = pre(j + 1) if j + 1 < NJ else None
            if nxt is not None:
                tile_hook[0] = lambda nxt=nxt: step(nxt)
            att(j)
            tile_hook[0] = None
            if nxt is not None:
                while step(nxt) != "done":
                    pass

    def softmax_head(j, qt, qkey, chunk, base, d, h, scale, bias_fn, ob, obk, qchunk=None, biaskey="FB"):
        qchunk = chunk if qchunk is None else qchunk
        nkt = 4 * (j + 1)
        first = True
        LOOK = 2
        pend = {}

        def qk(kt):
            sbk_t, sbk = Sb.next()
            P.op("pe", lambda e, sbk_t=sbk_t, kt=kt: e.matmul(sbk_t[:], lhsT=KT[base:base + d, chunk, kt * 128:(kt + 1) * 128],
                                                              rhs=qt[base:base + d, qchunk, :], start=True, stop=True),
                 reads=[("KT", chunk, kt // 4), qkey], writes=[sbk])
            pend[kt] = (sbk_t, sbk)

        for kt in range(min(LOOK, nkt)):
            qk(kt)
        for kt in range(nkt):
            sbk_t, sbk = pend.pop(kt)
            pt, ptk = PTs.next()
            if bias_fn is None:
                P.op("act", lambda e, sbk_t=sbk_t, pt=pt: e.activation(out=pt[:], in_=sbk_t[:], func=AF.Exp, scale=scale),
                     reads=[sbk], writes=[ptk])
            else:
                bap = bias_fn(kt)
                P.op("act", lambda e, sbk_t=sbk_t, pt=pt, bap=bap: e.activation(out=pt[:], in_=sbk_t[:], func=AF.Exp,
                                                                                 scale=scale, bias=bap),
                     reads=[sbk, biaskey], writes=[ptk])
            i = kt - 4 * j
            if i >= 0:
                P.op("pool", lambda e, pt=pt, i=i: e.tensor_tensor(out=pt[:, i * 128:(i + 1) * 128],
                                                                   in0=pt[:, i * 128:(i + 1) * 128], in1=mD[:],
                                                                   op=ALU.mult), reads=[ptk, "mD"], writes=[ptk])
            if kt + LOOK < nkt:
                qk(kt + LOOK)
            for qs in range(max(i, 0), 4):
                last = (kt == 4 * j + qs)
                P.op("pe", lambda e, pt=pt, qs=qs, kt=kt, first=first, last=last: e.matmul(
                    ob[:, qs * 65:(qs + 1) * 65], lhsT=pt[:, qs * 128:(qs + 1) * 128], rhs=V[:, kt, h, :],
                    start=first, stop=last, skip_group_check=True), reads=[ptk, ("V", kt // 4)], writes=[obk])
                first = False
            if tile_hook[0] is not None:
                tile_hook[0]()

    def normalize(ob, obk, dst_ap, dstkey, rcol):
        o3 = ob[:, 0:260].rearrange("p (a b) -> p a b", b=65)
        P.op("dve", lambda e: e.reciprocal(out=rinv[:, rcol:rcol + 4], in_=o3[:, :, 64]), reads=[obk], writes=[("rinv", rcol)])
        P.op("dve", lambda e: e.tensor_tensor(out=dst_ap, in0=o3[:, :, 0:64],
                                              in1=rinv[:, rcol:rcol + 4].unsqueeze(2).to_broadcast([128, 4, 64]),
                                              op=ALU.mult), reads=[obk, ("rinv", rcol)], writes=[dstkey])

    def stage_out(j, m, stg, stk):
        dst = T["mixed"][j * 512:(j + 1) * 512, m * 256:(m + 1) * 256].rearrange("(a p) f -> p a f", p=128)
        P.dma("sp", lambda e: e.dma_start(out=dst, in_=stg[:]), reads=[stk])

    def load_wm(c0, n):
        P.dma("pool", lambda e: e.dma_start(out=Wm[:, :, 0:n],
                                            in_=T["win"][:, c0:c0 + n].rearrange("(c p) n -> p c n", p=128)),
              writes=["Wm"])

    def sb_head(j, qt, qkey, chunk, base, h, stg, stk):
        scale = 0.125
        kts = list(range(4 * (j + 1) - 1, -1, -1))
        n = len(kts)
        rb, rbk = _ob[3]
        P.op("dve", lambda e: e.memset(osb[:], 0.0), writes=["osb"])
        P.op("dve", lambda e: e.memset(fbuf[:], 1.0), writes=["fbuf"])
        st = {}
        rfirst = [True]

        def S1(kt):
            i = kt - 4 * j
            zb, zk = Sb.next()
            e32, ek = E32.next()
            spb, spk = SPB.next()
            P.op("pe", lambda e, zb=zb, kt=kt: e.matmul(zb[:], lhsT=KT[:, chunk, kt * 128:(kt + 1) * 128],
                                                        rhs=qt[:, h, :], start=True, stop=True),
                 reads=[("KT", chunk, kt // 4), qkey], writes=[zk])
            P.op("act", lambda e, zb=zb, e32=e32: e.activation(out=e32[:], in_=zb[:], func=AF.Exp, scale=scale),
                 reads=[zk], writes=[ek])
            P.op("act", lambda e, e32=e32, spb=spb: e.activation(out=spb[:], in_=e32[:], func=AF.Ln, bias=1.0),
                 reads=[ek], writes=[spk])
            if i >= 0:
                if i > 0:
                    P.op("pool", lambda e, spb=spb, i=i: e.memset(spb[:, 0:i * 128], 0.0), writes=[spk])
                P.op("pool", lambda e, spb=spb, i=i: e.tensor_tensor(out=spb[:, i * 128:(i + 1) * 128],
                                                                     in0=spb[:, i * 128:(i + 1) * 128], in1=mS[:],
                                                                     op=ALU.mult), reads=[spk, "mS"], writes=[spk])
            st[kt] = dict(zb=zb, zk=zk, spb=spb, spk=spk, i=i)

        def S2(kt):
            d_ = st[kt]
            zb, zk, spb, spk, i = d_["zb"], d_["zk"], d_["spb"], d_["spk"], d_["i"]
            wb, wk = PTs.next()
            P.op("pe", lambda e, zb=zb, spb=spb: e.matmul(zb[:], lhsT=negtri[:], rhs=spb[:], start=False, stop=True,
                                                          skip_group_check=True), reads=[spk, "negtri"], writes=[zk])
            P.op("act", lambda e, zb=zb, wb=wb: e.activation(out=wb[:], in_=zb[:], func=AF.Exp, scale=scale),
                 reads=[zk], writes=[wk])
            if i >= 0:
                P.op("pool", lambda e, wb=wb, i=i: e.tensor_tensor(out=wb[:, i * 128:(i + 1) * 128],
                                                                   in0=wb[:, i * 128:(i + 1) * 128], in1=mS[:],
                                                                   op=ALU.mult), reads=[wk, "mS"], writes=[wk])
            d_["wb"], d_["wk"] = wb, wk

        def S3(kt):
            d_ = st.pop(kt)
            spb, spk, i, wb, wk = d_["spb"], d_["spk"], d_["i"], d_["wb"], d_["wk"]
            q0 = max(i, 0)
            ol, olk = ObSB.next()
            pfirst = True
            for qs in range(q0, 4):
                P.op("pe", lambda e, wb=wb, qs=qs, kt=kt, ol=ol, pfirst=pfirst: e.matmul(
                    ol[:, qs * 64:(qs + 1) * 64], lhsT=wb[:, qs * 128:(qs + 1) * 128], rhs=V[:, kt, h, 0:64],
                    start=pfirst, stop=True, skip_group_check=True), reads=[wk, ("V", kt // 4)], writes=[olk])
                pfirst = False
            fq0 = q0 + 1 if i >= 0 else 0
            if fq0 < 4 and not rfirst[0]:
                P.op("act", lambda e, fq0=fq0: e.activation(out=fbuf[:, fq0:4], in_=rb[:, fq0:4], func=AF.Exp, scale=-1.0),
                     reads=[rbk], writes=["fbuf"])
            o3 = ol[:, 0:256].rearrange("p (a b) -> p a b", b=64)
            P.op("dve", lambda e, o3=o3, q0=q0: e.tensor_tensor(
                out=tmpo[:, q0:4, :], in0=o3[:, q0:4, :],
                in1=fbuf[:, q0:4].unsqueeze(2).to_broadcast([128, 4 - q0, 64]), op=ALU.mult),
                 reads=[olk, "fbuf"], writes=["tmpo"])
            P.op("pool", lambda e, q0=q0: e.tensor_tensor(out=osb[:, q0:4, :], in0=osb[:, q0:4, :], in1=tmpo[:, q0:4, :],
                                                          op=ALU.add), reads=["tmpo", "osb"], writes=["osb"])
            for qs in range(q0, 4):
                P.op("pe", lambda e, spb=spb, qs=qs, rf=rfirst[0]: e.matmul(
                    rb[:, qs:qs + 1], lhsT=spb[:, qs * 128:(qs + 1) * 128], rhs=onesb[:, 0:1], start=rf, stop=True,
                    skip_group_check=True), reads=[spk, "onesb", "fbuf"], writes=[rbk])
                rfirst[0] = False

        for it in range(n + 2):
            if it < n:
                S1(kts[it])
            if 0 <= it - 1 < n:
                S2(kts[it - 1])
            if 0 <= it - 2 < n:
                S3(kts[it - 2])
        P.op("act", lambda e: e.copy(out=stg[:, :, h * 64:(h + 1) * 64], in_=osb[:]), reads=["osb"], writes=[stk])

    if stop_after < 1:
        return
    SKIP = os.environ.get("SKIPMIX", "")
    for stg_, stk_ in stage.items:
        P.op("pool", lambda e, stg_=stg_: e.memset(stg_[:], 0.0), writes=[stk_])
    P.op("dve", lambda e: e.memset(KT[:], 0.0), writes=[("KT", c_, j_) for c_ in range(4) for j_ in range(NJ)])
    zero_qt()
    load_wm(0, 768)
    for j in range(NJ if "0" not in SKIP else 0):
        qt = QT[j % 2]
        qkey = ("QT", j % 2)
        for c in range(max(1, NHP // 2)):
            proj_q_padded(j, c * 128, qt, qkey, c)
            proj_fm(j, 256 + c * 128, KT[:, c, j * 512:(j + 1) * 512], ("KT", c, j))
        proj_v(j, 512, 256)
        stg, stk = stage.next()
        for h in range(NHP):
            sb_head(j, qt, qkey, h // 2, 64 * (h % 2), h, stg, stk)
        stage_out(j, 0, stg, stk)

    if stop_after < 2:
        return
    zero_qt()
    load_wm(768, 1792)
    sc_d = 1.0 / math.sqrt(32.0)
    for j in range(NJ if "1" not in SKIP else 0):
        qt = QT[j % 2]
        qkey = ("QT", j % 2)
        rope_tables(j)
        for c in range((2 * NHP + 2) // 3):
            bA, kA = proj_fm(j, c * 128, None, None, post=True)
            bB, kB = proj_fm(j, 384 + c * 128, None, None, post=True)
            rope_evac(bA, kA, bB, kB, qtmp[0:96, :], "qtmp", 0, 96)
            for u_ in range(3 * c, min(3 * c + 3, 2 * NHP)):
                sl = 32 * (u_ % 3)
                P.op("dve", lambda e, u_=u_, sl=sl, qt=qt: e.tensor_copy(out=qt[sl:sl + 32, u_, :], in_=qtmp[sl:sl + 32, :]),
                     reads=["qtmp"], writes=[qkey])
            bA, kA = proj_fm(j, 768 + c * 128, None, None, post=True)
            bB, kB = proj_fm(j, 1152 + c * 128, None, None, post=True)
            rope_evac(bA, kA, bB, kB, KT[0:96, c, j * 512:(j + 1) * 512], ("KT", c, j), 0, 96)
        proj_v(j, 1536, 256)
        stg, stk = stage.next()
        for h in range(NHP):
            o1, o1k = Ob.next()
            o2, o2k = Ob.next()
            u1, u2 = 2 * h, 2 * h + 1
            softmax_head(j, qt, qkey, u1 // 3, 0, 128, h, sc_d, None, o1, o1k, qchunk=u1)
            softmax_head(j, qt, qkey, u2 // 3, 0, 128, h, sc_d, None, o2, o2k, qchunk=u2)
            normalize(o1, o1k, tmpo[:], "tmpo", 0)
            normalize(o2, o2k, tmpo2[:], "tmpo2", 4)
            P.op("dve", lambda e: e.scalar_tensor_tensor(out=tmpo[:], in0=tmpo2[:], scalar=lam[:, 0:1], in1=tmpo[:],
                                                         op0=ALU.mult, op1=ALU.add), reads=["tmpo", "tmpo2", "lam0"],
                 writes=["tmpo"])
            P.op("pool", lambda e: e.tensor_tensor(out=tmpo2[:], in0=tmpo[:], in1=tmpo[:], op=ALU.mult), reads=["tmpo"],
                 writes=["tmpo2"])
            P.op("dve", lambda e: e.reduce_sum(out=ssq[:, 0:4], in_=tmpo2[:], axis=AX.X), reads=["tmpo2"], writes=["ssq"])
            P.op("act", lambda e: e.activation(out=ssq[:, 0:4], in_=ssq[:, 0:4], func=AF.Ln, scale=1.0 / 64.0,
                                               bias=float(RMS_EPS)), reads=["ssq"], writes=["ssq"])
            P.op("act", lambda e: e.activation(out=ssq[:, 0:4], in_=ssq[:, 0:4], func=AF.Exp, scale=-0.5),
                 reads=["ssq"], writes=["ssq"])
            P.op("dve", lambda e: e.tensor_tensor(out=tmpo[:], in0=tmpo[:],
                                                  in1=ssq[:, 0:4].unsqueeze(2).to_broadcast([128, 4, 64]), op=ALU.mult),
                 reads=["tmpo", "ssq"], writes=["tmpo"])
            P.op("dve", lambda e, h=h, stg=stg: e.tensor_tensor(out=stg[:, :, h * 64:(h + 1) * 64], in0=tmpo[:], in1=dgb[:],
                                                       op=ALU.mult), reads=["tmpo", "dgb"], writes=[stk])
        stage_out(j, 1, stg, stk)

    if stop_after < 3:
        return
    zero_qt()
    load_wm(2560, 772)
    P.op("dve", lambda e: e.memset(Gc[:], 0.0), writes=["Gc"])
    def fox_pre(j):
        qt = QT[j % 2]
        qkey = ("QT", j % 2)
        FBj = FB[:, j % 2]
        fbk = ("FB", j % 2)
        for c in range(max(1, NHP // 2)):
            proj_q_padded(j, c * 128, qt, qkey, c)
            yield
            proj_fm(j, 256 + c * 128, KT[:, c, j * 512:(j + 1) * 512], ("KT", c, j))
            yield

        def gate_extra(t, bank, bk):
            P.op("dve", lambda e: e.tensor_tensor(out=xg[:, t, :], in0=bank[:, 256:260], in1=bfb[:], op=ALU.add),
                 reads=[bk, "bfb"], writes=["xg"])

        FOXMIN = int(os.environ.get("FOXMIN", "0"))
        proj_v(j, 512, 256)
        yield
        if FOXMIN < 1:
            gtb, gtk = pools["O"].next()
            for t in range(4):
                tt = 4 * j + t
                for kc in range(8):
                    P.op("pe", lambda e, kc=kc, tt=tt, t=t, gtb=gtb: e.matmul(gtb[:, t * 4:(t + 1) * 4],
                                                                             lhsT=hT[:, kc, tt * 128:(tt + 1) * 128],
                                                                             rhs=Wm[:, kc, 768:772], start=(kc == 0),
                                                                             stop=(kc == 7), skip_group_check=True),
                         reads=["Wm", ("hT", j)], writes=[gtk])
            P.op("dve", lambda e, gtb=gtb: e.tensor_tensor(out=xg[:], in0=gtb[:, 0:16].rearrange("p (a b) -> p a b", b=4),
                                                           in1=bfb[:].unsqueeze(1).to_broadcast([128, 4, 4]), op=ALU.add),
                 reads=[gtk, "bfb"], writes=["xg"])
        if FOXMIN < 1 and not os.environ.get("FOXNOACT"):
            P.op("act", lambda e: e.activation(out=lgt[:], in_=xg[:], func=AF.Exp, scale=-1.0), reads=["xg"], writes=["lgt"])
            P.op("act", lambda e: e.activation(out=lgt[:], in_=lgt[:], func=AF.Ln, bias=1.0), reads=["lgt"], writes=["lgt"])
        if os.environ.get("FOXNOG"):
            P.op("dve", lambda e: e.memset(G[:], 0.5), writes=["G"])
            P.op("dve", lambda e: e.memset(Gmid[:], 0.25), writes=["Gmid"])
        else:
            l16 = lgt[:].rearrange("p a b -> p (a b)")
            P.op("dve", lambda e: e.tensor_copy(out=lsp[:, :, 0, :], in_=lgt[:]), reads=["lgt"], writes=["lsp0"])
            P.op("dve", lambda e: e.tensor_tensor(out=lr1[:], in0=lgt[:], in1=lsp[:, :, 0, :], op=ALU.subtract),
                 reads=["lgt", "lsp0"], writes=["lr1"])
            P.op("dve", lambda e: e.tensor_copy(out=lsp[:, :, 1, :], in_=lr1[:]), reads=["lr1"], writes=["lsp1"])
            P.op("dve", lambda e: e.tensor_tensor(out=lr2[:], in0=lr1[:], in1=lsp[:, :, 1, :], op=ALU.subtract),
                 reads=["lr1", "lsp1"], writes=["lr2"])
            P.op("dve", lambda e: e.tensor_copy(out=lsp[:, :, 2, :], in_=lr2[:]), reads=["lr2"], writes=["lsp2"])
            LS = ["lsp0", "lsp1", "lsp2"]
            gb, gbk = pools["O"].next()
            for t in range(4):
                P.op("pe", lambda e, t=t: e.matmul(gb[:, t * 12:(t + 1) * 12], lhsT=trib[:],
                                                   rhs=lsp[:, t, :, :].rearrange("p a b -> p (a b)"), start=True,
                                                   stop=(t == 0), skip_group_check=True), reads=LS + ["trib"], writes=[gbk])
                for t2 in range(t):
                    P.op("pe", lambda e, t=t, t2=t2: e.matmul(gb[:, t * 12:(t + 1) * 12], lhsT=onesb[:],
                                                              rhs=lsp[:, t2, :, :].rearrange("p a b -> p (a b)"),
                                                              start=False, stop=(t2 == t - 1), skip_group_check=True),
                         reads=LS + ["onesb"], writes=[gbk])
            P.op("dve", lambda e: e.reduce_sum(out=lr1[:], in_=gb[:, 0:48].rearrange("p (t s h) -> p t h s", s=3, h=4),
                                               axis=AX.X), reads=[gbk], writes=["lr1"])
            P.op("dve", lambda e, j=j: e.tensor_tensor(out=G[:, 4 * j:4 * j + 4, :], in0=lr1[:],
                                                       in1=Gc[:].unsqueeze(1).to_broadcast([128, 4, 4]), op=ALU.add),
                 reads=["lr1", "Gc"], writes=["G"])
            cb, cbk = pools["O"].next()
            for t in range(4):
                P.op("pe", lambda e, t=t: e.matmul(cb[:, 0:12], lhsT=onesb[:], rhs=lsp[:, t, :, :].rearrange("p a b -> p (a b)"),
                                                   start=(t == 0), stop=(t == 3), skip_group_check=True),
                     reads=LS + ["onesb"], writes=[cbk])
            for t in range(3):
                P.op("pe", lambda e, t=t: e.matmul(cb[:, 12:24], lhsT=(onesb[:] if t < 2 else e0b[:]),
                                                   rhs=lsp[:, t, :, :].rearrange("p a b -> p (a b)"), start=False,
                                                   stop=(t == 2), skip_group_check=True), reads=LS + ["onesb", "e0b"],
                     writes=[cbk])
            P.op("dve", lambda e: e.reduce_sum(out=lr2[:, 0:2, :], in_=cb[:, 0:24].rearrange("p (t s h) -> p t h s", s=3, h=4),
                                               axis=AX.X), reads=[cbk], writes=["lr2"])
            P.op("dve", lambda e: e.tensor_tensor(out=Gmid[:], in0=lr2[:, 1, :], in1=Gc[:], op=ALU.add), reads=["lr2", "Gc"],
                 writes=["Gmid"])
            P.op("dve", lambda e: e.tensor_tensor(out=Gc[:], in0=lr2[:, 0, :], in1=Gc[:], op=ALU.add), reads=["lr2", "Gc"],
                 writes=["Gc"])
        nkt = 4 * (j + 1)
        for h in range(NHP if FOXMIN < 2 else 0):
            P.op("dve", lambda e, h=h, nkt=nkt, FBj=FBj: e.tensor_scalar(out=FBj[:, h, 0:nkt], in0=G[:, 0:nkt, h],
                                                                         scalar1=Gmid[:, h:h + 1], scalar2=60.0,
                                                                         op0=ALU.subtract, op1=ALU.min),
                 reads=["G", "Gmid"], writes=[fbk])
        yield

    def fox_att(j):
        qt = QT[j % 2]
        qkey = ("QT", j % 2)
        FBj = FB[:, j % 2]
        fbk = ("FB", j % 2)
        stg, stk = stage.next()
        for h in range(NHP):
            ob, obk = ObA.next()
            softmax_head(j, qt, qkey, h // 2, 0, 128, h, 0.125, (lambda kt, h=h, FBj=FBj: FBj[:, h, kt:kt + 1]), ob, obk,
                         qchunk=h, biaskey=fbk)
            normalize(ob, obk, tmpo[:], "tmpo", 0)
            P.op("act", lambda e, h=h, stg=stg: e.copy(out=stg[:, :, h * 64:(h + 1) * 64], in_=tmpo[:]), reads=["tmpo"],
                 writes=[stk])
        stage_out(j, 2, stg, stk)

    run_pipelined(fox_pre, fox_att)

    if stop_after < 4:
        return
    load_wm(3332, 640)
    sc_m = 1.0 / math.sqrt(96.0)
    def mla_pre(j):
        qt = QT[j % 2]
        qkey = ("QT", j % 2)
        rope_tables(j)
        yield
        for t in range(4):
            tt = 4 * j + t
            bank, bk = pools["O"].next()
            for kc in range(8):
                P.op("pe", lambda e, kc=kc, bank=bank, tt=tt: e.matmul(bank[:, 0:384], lhsT=hT[:, kc, tt * 128:(tt + 1) * 128],
                                                                       rhs=Wm[:, kc, 0:384], start=(kc == 0),
                                                                       stop=(kc == 7)),
                     reads=["Wm", ("hT", j)], writes=[bk])
            P.op("act", lambda e, bank=bank: e.activation(out=junk[:, 0:256], in_=bank[:, 0:256], func=AF.Square,
                                                          accum_out=ssq[:, 4:5]), reads=[bk], writes=["junk", "ssq4"])
            P.op("act", lambda e, bank=bank: e.activation(out=junk[:, 0:128], in_=bank[:, 256:384], func=AF.Square,
                                                          accum_out=ssq[:, 5:6]), reads=[bk], writes=["junk", "ssq5"])
            P.op("act", lambda e: e.activation(out=ssq[:, 4:5], in_=ssq[:, 4:5], func=AF.Ln, scale=1.0 / 256.0,
                                               bias=float(RMS_EPS)), reads=["ssq4"], writes=["ssq4"])
            P.op("act", lambda e: e.activation(out=ssq[:, 5:6], in_=ssq[:, 5:6], func=AF.Ln, scale=1.0 / 128.0,
                                               bias=float(RMS_EPS)), reads=["ssq5"], writes=["ssq5"])
            P.op("act", lambda e: e.activation(out=ssq[:, 4:6], in_=ssq[:, 4:6], func=AF.Exp, scale=-0.5),
                 reads=["ssq4", "ssq5"], writes=["ssq4", "ssq5"])
            P.op("dve", lambda e, bank=bank, t=t: e.scalar_tensor_tensor(out=cqs[:, t, 0:256], in0=bank[:, 0:256],
                                                                         scalar=ssq[:, 4:5], in1=qgb[:], op0=ALU.mult,
                                                                         op1=ALU.mult), reads=[bk, "ssq4", "qgb"],
                 writes=[("cqs", t)])
            P.op("dve", lambda e, bank=bank, t=t: e.scalar_tensor_tensor(out=cqs[:, t, 256:384], in0=bank[:, 256:384],
                                                                         scalar=ssq[:, 5:6], in1=kvgb[:], op0=ALU.mult,
                                                                         op1=ALU.mult), reads=[bk, "ssq5", "kvgb"],
                 writes=[("cqs", t)])
            for c in range(3):
                P.op("pe", lambda e, t=t, c=c: e.transpose(out=Tb[:, c, :], in_=cqs[:, t, c * 128:(c + 1) * 128],
                                                           identity=identb[:]), reads=[("cqs", t), "identb"],
                     writes=["Tb"])
            P.op("dve", lambda e, t=t: e.tensor_copy(out=cqT[:, :, t * 128:(t + 1) * 128], in_=Tb[:, 0:3, :]),
                 reads=["Tb"], writes=["cqT"])
            yield
        for t in range(4):
            tt = 4 * j + t
            bank, bk = pools["O"].next()
            P.op("pe", lambda e, bank=bank, t=t: e.matmul(bank[:, 0:256], lhsT=cqT[:, 2, t * 128:(t + 1) * 128],
                                                          rhs=Wukv[:, 512:768], start=True, stop=True),
                 reads=["cqT", "Wukv"], writes=[bk])
            P.op("act", lambda e, bank=bank, tt=tt: e.copy(out=V[:, tt, :, 0:64],
                                                           in_=bank[:, 0:256].rearrange("p (a b) -> p a b", b=64)),
                 reads=[bk, "Vall"], writes=[("V", j)])
            yield
        bA, kA = proj_fm(j, 384, None, None, post=True)
        bB, kB = proj_fm(j, 512, None, None, post=True)
        rope_evac(bA, kA, bB, kB, krope[64:96, :], "krope", 64, 96)
        for h in range(NHP):
            bank, bk = pools["S"].next()
            P.op("pe", lambda e, bank=bank, h=h: e.matmul(bank[:], lhsT=Wukv[:, h * 128:(h + 1) * 128], rhs=cqT[:, 2, :],
                                                          start=True, stop=True), reads=["cqT", "Wukv"], writes=[bk])
            P.op("act", lambda e, bank=bank, h=h, j=j: e.copy(out=KT[0:64, h, j * 512:(j + 1) * 512], in_=bank[0:64, :]),
                 reads=[bk], writes=[("KT", h, j)])
            P.op("pool", lambda e, h=h, j=j: e.tensor_copy(out=KT[64:96, h, j * 512:(j + 1) * 512], in_=krope[64:96, :]),
                 reads=["krope", ("KT", h, j)], writes=[("KT", h, j)])
            bA, kA = pools["S"].next()
            bB, kB = pools["S"].next()
            for c2 in range(2):
                P.op("pe", lambda e, bA=bA, c2=c2, h=h: e.matmul(bA[:], lhsT=Wuq[:, c2, h * 256:h * 256 + 128],
                                                                 rhs=cqT[:, c2, :], start=(c2 == 0), stop=(c2 == 1)),
                     reads=["cqT", "Wuq"], writes=[kA])
            for c2 in range(2):
                P.op("pe", lambda e, bB=bB, c2=c2, h=h: e.matmul(bB[:], lhsT=Wuq[:, c2, h * 256 + 128:h * 256 + 256],
                                                                 rhs=cqT[:, c2, :], start=(c2 == 0), stop=(c2 == 1)),
                     reads=["cqT", "Wuq"], writes=[kB])
            P.op("act", lambda e, bA=bA, h=h, qt=qt: e.copy(out=qt[0:64, h, :], in_=bA[0:64, :]), reads=[kA], writes=[qkey])
            rope_evac(bA, kA, bB, kB, qt[64:96, h, :], qkey, 64, 96)
            yield

    def mla_att(j):
        qt = QT[j % 2]
        qkey = ("QT", j % 2)
        stg, stk = stage.next()
        for h in range(NHP):
            ob, obk = ObA.next()
            softmax_head(j, qt, qkey, h, 0, 96, h, sc_m, None, ob, obk)
            normalize(ob, obk, tmpo[:], "tmpo", 0)
            P.op("act", lambda e, h=h, stg=stg: e.copy(out=stg[:, :, h * 64:(h + 1) * 64], in_=tmpo[:]), reads=["tmpo"],
                 writes=[stk])
        stage_out(j, 3, stg, stk)

    run_pipelined(mla_pre, mla_att)


def declare_a_inputs(nc, S, pre_ln, sfx=""):
    T = {}

    def din(name, shape, dt=F32):
        T[name] = nc.dram_tensor(name + sfx, list(shape), dt, kind="ExternalInput").ap()

    din("hin", [S, D])
    din("pos", [S], I32)
    din("win", [D, NWIN])
    din("wuq", [256, 1024])
    din("wukv", [128, 768])
    din("bf", [4])
    din("dlam", [128])
    din("dg", [64])
    din("qg", [256])
    din("kvg", [128])
    if pre_ln:
        din("lng", [D])
        din("lnb", [D])
    return T


def declare_consts(nc, T):
    for name, shape in (("ident", [128, 128]), ("maskd", [128, 128]), ("masks", [128, 128]), ("negtri", [128, 128]),
                        ("triincl", [128, 128]), ("ones", [128, 128]), ("e0", [128, 128]), ("ropef", [128, 2]),
                        ("iota_e", [128, NE]), ("tristrict", [128, 128])):
        T[name] = nc.dram_tensor(name, shape, F32, kind="ExternalInput").ap()


def build_a(S, pre_ln, lam_init, stop_after=9, NHP=4):
    nc = bass.Bass("TRN2", target_bir_lowering=False)
    T = declare_a_inputs(nc, S, pre_ln)
    declare_consts(nc, T)
    T["mixed"] = nc.dram_tensor("mixed", [S, D], BF16, kind="ExternalOutput").ap()
    if pre_ln:
        T["h0"] = nc.dram_tensor("h0", [S, D], F32, kind="ExternalOutput").ap()
    with ExitStack() as es:
        P = Prog(nc, es)
        phase_a(nc, P, es, S, T, pre_ln, lam_init, "a_", stop_after, NHP)
        P.finish("sp")
        P.emit()
    return nc


def a_inputs(l, inputs, hin, pos, pre_ln, hp=(0, 1, 2, 3)):
    m = dict(hin=np.ascontiguousarray(hin, dtype=np.float32), pos=np.ascontiguousarray(pos, dtype=np.int32),
             win=prep_w_in(np.asarray(inputs["w_in"][l]), hp), wuq=prep_w_uq(np.asarray(inputs["mla_w_uq"][l]), hp),
             wukv=prep_w_ukv(np.asarray(inputs["mla_w_ukv"][l]), hp),
             bf=np.ascontiguousarray(np.asarray(inputs["b_forget"][l])[list(hp)], dtype=np.float32),
             dlam=np.ascontiguousarray(np.asarray(inputs["diff_lambda"][l]).reshape(128), dtype=np.float32),
             dg=np.ascontiguousarray(inputs["diff_subln_g"][l], dtype=np.float32),
             qg=np.ascontiguousarray(inputs["mla_q_norm_g"][l], dtype=np.float32),
             kvg=np.ascontiguousarray(inputs["mla_kv_norm_g"][l], dtype=np.float32))
    if pre_ln:
        m["lng"] = np.ascontiguousarray(inputs["ln_in_g"], dtype=np.float32)
        m["lnb"] = np.ascontiguousarray(inputs["ln_in_b"], dtype=np.float32)
    m.update(host_consts())
    return m


def phase_b(nc, P, es, NTOK, C, T, tag, stop_after=9):
    NTT = NTOK // 128
    CT = C // 128
    NSLOT = NE * C
    RNG = [(n0, min(512, C - n0)) for n0 in range(0, C, 512)]

    es1, es2, es3 = ExitStack(), ExitStack(), ExitStack()
    cur = [es]

    def sb(name, shape, dt):
        return cur[0].enter_context(nc.sbuf_tensor(tag + name, shape, dt))

    def ps(name, shape, dt):
        return es.enter_context(nc.psum_tensor(tag + name, shape, dt))

    K_ = lambda s_: tag + s_
    identb = sb("identb", [128, 128], BF16)
    onesb = sb("onesb", [128, 128], BF16)
    tsb = sb("tsb", [128, 128], BF16)
    ecap = sb("ecap", [128, NE], F32)
    bguT = sb("bguT", [128, 16, NE], F32)
    bguT2 = sb("bguT2", [128, 16, NE], F32)
    cnt = sb("cnt", [128, NE], F32)
    slotk = sb("slotk", [128, NTT, 4], F32)
    gk = sb("gk", [128, NTT, 4], F32)
    idx = sb("idx", [128, NTT, 4], I32)
    zer = sb("zer", [128, 1024], F32)
    st6 = sb("st6", [128, 2, 6], F32)
    mv = sb("mv", [128, 2], F32)
    ridx = sb("ridx", [128, NTT], I32)
    ridg0 = sb("ridg0", [128, NTT], I32)
    ridg1 = sb("ridg1", [128, NTT], I32)
    cur[0] = es1
    Wout = sb("Wout", [128, 8, 1024], BF16)
    g1b = sb("g1b", [128, 1024], F32)
    b1b = sb("b1b", [128, 1024], F32)
    rbb = sb("rbb", [128, NE], F32)
    rwf = sb("rwf", [128, 8, NE], F32)
    rwr = sb("rwr", [128, 8, NE], F32)
    rw0 = sb("rw0", [128, 8, NE], BF16)
    rw1 = sb("rw1", [128, 8, NE], BF16)
    bguf = sb("bguf", [NE, 2048], F32)
    bgur = sb("bgur", [NE, 2048], F32)
    bgu0 = sb("bgu0", [NE, 2048], BF16)
    bgu1 = sb("bgu1", [NE, 2048], BF16)
    mtl = Rot([(sb(f"mt{i}", [128, 1024], BF16), K_(f"mt{i}")) for i in range(4)])
    mTs = Rot([(sb(f"mT{i}", [128, 8, 128], BF16), K_(f"mT{i}")) for i in range(2)])
    hts = Rot([(sb(f"ht{i}", [128, 1024], F32), K_(f"ht{i}")) for i in range(2)])
    rts = Rot([(sb(f"rt{i}", [128, 1024], F32), K_(f"rt{i}")) for i in range(2)])
    h1bs = Rot([(sb(f"h1b{i}", [128, 1024], BF16), K_(f"h1b{i}")) for i in range(2)])
    h1m = sb("h1m", [128, 1024], BF16)
    hres_ = sb("hres_", [128, 1024], F32)
    a0T = sb("a0T", [128, 8, 128], BF16)
    a1T = sb("a1T", [128, 8, 128], BF16)
    lg = sb("lg", [128, NE], F32)
    mx8 = sb("mx8", [128, 8], F32)
    msk = sb("msk", [128, NE], F32)
    mskb = sb("mskb", [128, NE], BF16)
    nm = sb("nm", [128, 1], F32)
    ex = sb("ex", [128, NE], F32)
    ssum = sb("ssum", [128, 1], F32)
    gate = sb("gate", [128, NE], F32)
    pos = sb("pos", [128, NE], F32)
    okm = sb("okm", [128, NE], F32)
    slot = sb("slot", [128, NE], F32)
    sel = sb("sel", [128, NE], F32)
    tmp32 = sb("tmp32", [128, NE], F32)
    Sb = Rot([(ps(f"S{i}", [128, 512], F32), K_(f"S{i}")) for i in range(3)])
    Ob = Rot([(ps(f"O{i}", [128, 512], F32), K_(f"O{i}")) for i in range(4)])
    Tb = ps("Tb", [128, 8, 128], BF16)
    TbK = K_("Tb")

    def ld(q, dst, src, key):
        P.dma(q, lambda e: e.dma_start(out=dst, in_=src), writes=[key])

    ld("pool", identb[:], T["ident"], K_("identb"))
    ld("pool", onesb[:], T["ones"], K_("onesb"))
    ld("pool", tsb[:], T["tristrict"], K_("tsb"))
    ld("sp", ecap[:], T["iota_e"], K_("ecap"))
    ld("sp", g1b[:], T["ln1g"].partition_broadcast(128), K_("g1b"))
    ld("sp", b1b[:], T["ln1b"].partition_broadcast(128), K_("b1b"))
    ld("sp", rbb[:], T["rb"].partition_broadcast(128), K_("rbb"))
    ld("sp", rwf[:], T["rw"].rearrange("(c p) n -> p c n", p=128), K_("rwf"))
    ld("sp", bguf[:], T["bgu"], K_("bguf"))
    ld("pool", Wout[:], T["wout"].rearrange("(c p) n -> p c n", p=128), K_("Wout"))
    P.op("dve", lambda e: e.tensor_scalar(out=ecap[:], in0=ecap[:], scalar1=float(C), scalar2=None, op0=ALU.mult),
         reads=[K_("ecap")], writes=[K_("ecap")])
    P.op("dve", lambda e: e.memset(cnt[:], 0.0), writes=[K_("cnt")])
    P.op("dve", lambda e: e.memset(zer[:], 0.0), writes=[K_("zer")])
    P.dma("sp", lambda e: e.dma_start(out=T["ys"][NSLOT:NSLOT + 128, :], in_=zer[:]), reads=[K_("zer")], writes=[K_("ysd")])
    zb16 = zer[:].bitcast(BF16)
    for e_ in range(NE):
        for a_ in range(0, CT, 2):
            na = min(2, CT - a_)
            r0 = e_ * C + a_ * 128
            P.dma("sp", lambda e, r0=r0, na=na: e.dma_start(
                out=T["xs"][r0:r0 + na * 128, :].rearrange("(a p) f -> p a f", p=128),
                in_=zb16[:, 0:na * 1024].rearrange("p (a f) -> p a f", f=1024)), reads=[K_("zer")], writes=[K_(f"xsz{e_}_{a_}")])
    XSZ = [K_(f"xsz{e_}_{a_}") for e_ in range(NE) for a_ in range(0, CT, 2)]
    P.op("dve", lambda e: e.tensor_copy(out=rw0[:], in_=rwf[:]), reads=[K_("rwf")], writes=[K_("rw0")])
    P.op("dve", lambda e: e.tensor_tensor(out=rwr[:], in0=rwf[:], in1=rw0[:], op=ALU.subtract),
         reads=[K_("rwf"), K_("rw0")], writes=[K_("rwr")])
    P.op("dve", lambda e: e.tensor_copy(out=rw1[:], in_=rwr[:]), reads=[K_("rwr")], writes=[K_("rw1")])
    P.op("dve", lambda e: e.tensor_copy(out=bgu0[:], in_=bguf[:]), reads=[K_("bguf")], writes=[K_("bgu0")])
    P.op("dve", lambda e: e.tensor_tensor(out=bgur[:], in0=bguf[:], in1=bgu0[:], op=ALU.subtract),
         reads=[K_("bguf"), K_("bgu0")], writes=[K_("bgur")])
    P.op("dve", lambda e: e.tensor_copy(out=bgu1[:], in_=bgur[:]), reads=[K_("bgur")], writes=[K_("bgu1")])
    for part, (src, srck, dst) in enumerate(((bgu0, "bgu0", bguT), (bgu1, "bgu1", bguT2))):
        for g_ in range(2):
            for c in range(8):
                cc = g_ * 8 + c
                P.op("pe", lambda e, src=src, c=c, cc=cc: e.transpose(out=Tb[:, c, 0:NE], in_=src[:, cc * 128:(cc + 1) * 128],
                                                                      identity=identb[0:NE, 0:NE]),
                     reads=[K_(srck), K_("identb")], writes=[TbK])
            P.op("dve", lambda e, dst=dst, g_=g_: e.tensor_copy(out=dst[:, g_ * 8:(g_ + 1) * 8, :], in_=Tb[:, :, 0:NE]),
                 reads=[TbK], writes=[K_("bguT%d" % part)])
    P.op("dve", lambda e: e.tensor_tensor(out=bguT[:], in0=bguT[:], in1=bguT2[:], op=ALU.add),
         reads=[K_("bguT0"), K_("bguT1")], writes=[K_("bguT0")])

    def layer_norm_tile(x, xk, gb, bb, gk_, bk_):
        for c in range(2):
            P.op("dve", lambda e, c=c: e.bn_stats(out=st6[:, c, :], in_=x[:, c * 512:(c + 1) * 512]), reads=[xk],
                 writes=[K_("st6")])
        P.op("dve", lambda e: e.bn_aggr(out=mv[:], in_=st6[:].rearrange("p a b -> p (a b)")), reads=[K_("st6")],
             writes=[K_("mv")])
        P.op("act", lambda e: e.activation(out=mv[:, 1:2], in_=mv[:, 1:2], func=AF.Ln, bias=float(LN_EPS)),
             reads=[K_("mv")], writes=[K_("mv")])
        P.op("act", lambda e: e.activation(out=mv[:, 1:2], in_=mv[:, 1:2], func=AF.Exp, scale=-0.5), reads=[K_("mv")],
             writes=[K_("mv")])
        P.op("dve", lambda e: e.scalar_tensor_tensor(out=x[:], in0=x[:], scalar=mv[:, 0:1], in1=gb[:], op0=ALU.subtract,
                                                     op1=ALU.mult), reads=[xk, K_("mv"), gk_], writes=[xk])
        P.op("dve", lambda e: e.scalar_tensor_tensor(out=x[:], in0=x[:], scalar=mv[:, 1:2], in1=bb[:], op0=ALU.mult,
                                                     op1=ALU.add), reads=[xk, K_("mv"), bk_], writes=[xk])

    use_idx = "rowidx" in T
    if use_idx:
        ld("sp", ridx[:], T["rowidx"], K_("ridx"))
    use_g = "mixG" in T
    if use_g:
        ld("sp", ridg0[:], T["rowidxg0"], K_("ridg0"))
        ld("sp", ridg1[:], T["rowidxg1"], K_("ridg1"))
    for t in range(NTT):
        mt, mk = mtl.next()
        mT, mTk = mTs.next()
        ht, hk = hts.next()
        rt, rk = rts.next()
        h1b, h1bk = h1bs.next()
        if use_g:
            mt1, mk1 = mtl.next()
            P.dma("pool", lambda e, mt=mt, t=t: e.indirect_dma_start(
                out=mt[:], out_offset=None, in_=T["mixG"],
                in_offset=bass.IndirectOffsetOnAxis(ap=ridg0[:, t:t + 1], axis=0)), reads=[K_("ridg0")], writes=[mk])
            P.dma("pool", lambda e, mt1=mt1, t=t: e.indirect_dma_start(
                out=mt1[:], out_offset=None, in_=T["mixG"],
                in_offset=bass.IndirectOffsetOnAxis(ap=ridg1[:, t:t + 1], axis=0)), reads=[K_("ridg1")], writes=[mk1])
        elif use_idx:
            P.dma("pool", lambda e, mt=mt, t=t: e.indirect_dma_start(
                out=mt[:], out_offset=None, in_=T["mixed"],
                in_offset=bass.IndirectOffsetOnAxis(ap=ridx[:, t:t + 1], axis=0)), reads=[K_("ridx")], writes=[mk])
        else:
            P.dma("sp", lambda e, mt=mt, t=t: e.dma_start(out=mt[:], in_=T["mixed"][t * 128:(t + 1) * 128, :]), writes=[mk])
        if "hres_local" in T:
            P.dma("sp", lambda e, ht=ht, t=t: e.dma_start(out=ht[:], in_=T["hres_local"][t * 128:(t + 1) * 128, :]),
                  writes=[hk])
        elif use_idx:
            P.dma("pool", lambda e, ht=ht, t=t: e.indirect_dma_start(
                out=ht[:], out_offset=None, in_=T["hres"],
                in_offset=bass.IndirectOffsetOnAxis(ap=ridx[:, t:t + 1], axis=0)), reads=[K_("ridx")], writes=[hk])
        else:
            P.dma("sp", lambda e, ht=ht, t=t: e.dma_start(out=ht[:], in_=T["hres"][t * 128:(t + 1) * 128, :]), writes=[hk])
        for c in range(8):
            if use_g:
                srct, srck = (mt, mk) if c % 2 == 0 else (mt1, mk1)
                c0 = (c // 2) * 256
            else:
                srct, srck, c0 = mt, mk, c * 128
            P.op("pe", lambda e, srct=srct, c=c, c0=c0: e.transpose(out=Tb[:, c, :], in_=srct[:, c0:c0 + 128],
                                                                    identity=identb[:]),
                 reads=[srck, K_("identb")], writes=[TbK])
        P.op("act", lambda e, mT=mT: e.copy(out=mT[:], in_=Tb[:]), reads=[TbK], writes=[mTk])
        for half in range(2):
            bank, bk = Sb.next()
            for c in range(8):
                P.op("pe", lambda e, bank=bank, c=c, half=half, mT=mT: e.matmul(
                    bank[:], lhsT=mT[:, c, :], rhs=Wout[:, c, half * 512:(half + 1) * 512], start=(c == 0), stop=(c == 7)),
                    reads=[mTk, K_("Wout")], writes=[bk])
            P.op("dve", lambda e, bank=bank, half=half, ht=ht, rt=rt: e.scalar_tensor_tensor(
                out=rt[:, half * 512:(half + 1) * 512], in0=ht[:, half * 512:(half + 1) * 512], scalar=float(ALPHA),
                in1=bank[:], op0=ALU.mult, op1=ALU.add), reads=[bk, hk], writes=[rk])
        layer_norm_tile(rt, rk, g1b, b1b, K_("g1b"), K_("b1b"))
        P.dma("act", lambda e, rt=rt, t=t: e.dma_start(out=T["h1s"][t * 128:(t + 1) * 128, :], in_=rt[:]), reads=[rk])
        P.op("act", lambda e, rt=rt, h1b=h1b: e.copy(out=h1b[:], in_=rt[:]), reads=[rk], writes=[h1bk])
        P.op("dve", lambda e, rt=rt, h1b=h1b: e.tensor_tensor(out=hres_[:], in0=rt[:], in1=h1b[:], op=ALU.subtract),
             reads=[rk, h1bk], writes=[K_("hres_")])
        P.op("act", lambda e: e.copy(out=h1m[:], in_=hres_[:]), reads=[K_("hres_")], writes=[K_("h1m")])
        for src, srck, dst, dstk in ((h1b, h1bk, a0T, K_("a0T")), (h1m, K_("h1m"), a1T, K_("a1T"))):
            for c in range(8):
                P.op("pe", lambda e, src=src, c=c: e.transpose(out=Tb[:, c, :], in_=src[:, c * 128:(c + 1) * 128],
                                                               identity=identb[:]), reads=[srck, K_("identb")], writes=[TbK])
            P.op("dve", lambda e, dst=dst: e.tensor_copy(out=dst[:], in_=Tb[:]), reads=[TbK], writes=[dstk])
        lb, lbk = Ob.next()
        n_mm = 0
        for (aT, aTk, rwx, rwk) in ((a0T, K_("a0T"), rw0, K_("rw0")), (a0T, K_("a0T"), rw1, K_("rw1")),
                                    (a1T, K_("a1T"), rw0, K_("rw0"))):
            for c in range(8):
                P.op("pe", lambda e, aT=aT, rwx=rwx, c=c, lb=lb, n_mm=n_mm: e.matmul(
                    lb[:, 0:NE], lhsT=aT[:, c, :], rhs=rwx[:, c, :], start=(n_mm == 0), stop=(n_mm == 23)),
                    reads=[aTk, rwk], writes=[lbk])
                n_mm += 1
        P.op("dve", lambda e, lb=lb: e.tensor_tensor(out=lg[:], in0=lb[:, 0:NE], in1=rbb[:], op=ALU.add),
             reads=[lbk, K_("rbb")], writes=[K_("lg")])
        P.op("dve", lambda e: e.max(out=mx8[:], in_=lg[:]), reads=[K_("lg")], writes=[K_("mx8")])
        P.op("dve", lambda e: e.tensor_scalar(out=msk[:], in0=lg[:], scalar1=mx8[:, 3:4], scalar2=None, op0=ALU.is_ge),
             reads=[K_("lg"), K_("mx8")], writes=[K_("msk")])
        P.op("dve", lambda e: e.tensor_scalar(out=nm[:], in0=mx8[:, 0:1], scalar1=-1.0, scalar2=None, op0=ALU.mult),
             reads=[K_("mx8")], writes=[K_("nm")])
        P.op("act", lambda e: e.activation(out=ex[:], in_=lg[:], func=AF.Exp, bias=nm[:, 0:1], scale=1.0),
             reads=[K_("lg"), K_("nm")], writes=[K_("ex")])
        P.op("dve", lambda e: e.tensor_tensor(out=ex[:], in0=ex[:], in1=msk[:], op=ALU.mult), reads=[K_("ex"), K_("msk")],
             writes=[K_("ex")])
        P.op("dve", lambda e: e.reduce_sum(out=ssum[:], in_=ex[:], axis=AX.X), reads=[K_("ex")], writes=[K_("ssum")])
        P.op("dve", lambda e: e.reciprocal(out=ssum[:], in_=ssum[:]), reads=[K_("ssum")], writes=[K_("ssum")])
        P.op("dve", lambda e: e.tensor_scalar(out=gate[:], in0=ex[:], scalar1=ssum[:, 0:1], scalar2=None, op0=ALU.mult),
             reads=[K_("ex"), K_("ssum")], writes=[K_("gate")])
        P.op("act", lambda e: e.copy(out=mskb[:], in_=msk[:]), reads=[K_("msk")], writes=[K_("mskb")])
        cb, cbk = Ob.next()
        P.op("pe", lambda e, cb=cb: e.matmul(cb[:, 0:NE], lhsT=tsb[:], rhs=mskb[:], start=True, stop=True),
             reads=[K_("tsb"), K_("mskb")], writes=[cbk])
        P.op("pe", lambda e, cb=cb: e.matmul(cb[:, NE:2 * NE], lhsT=onesb[:], rhs=mskb[:], start=False, stop=True,
                                             skip_group_check=True), reads=[K_("onesb"), K_("mskb")], writes=[cbk])
        P.op("dve", lambda e, cb=cb: e.tensor_tensor(out=pos[:], in0=cb[:, 0:NE], in1=cnt[:], op=ALU.add),
             reads=[cbk, K_("cnt")], writes=[K_("pos")])
        P.op("dve", lambda e, cb=cb: e.tensor_tensor(out=cnt[:], in0=cb[:, NE:2 * NE], in1=cnt[:], op=ALU.add),
             reads=[cbk, K_("cnt")], writes=[K_("cnt")])
        P.op("dve", lambda e: e.tensor_scalar(out=okm[:], in0=pos[:], scalar1=float(C), scalar2=None, op0=ALU.is_lt),
             reads=[K_("pos")], writes=[K_("okm")])
        P.op("dve", lambda e: e.tensor_tensor(out=slot[:], in0=pos[:], in1=ecap[:], op=ALU.add),
             reads=[K_("pos"), K_("ecap")], writes=[K_("slot")])
        P.op("dve", lambda e: e.tensor_scalar(out=slot[:], in0=slot[:], scalar1=-float(NSLOT), scalar2=None, op0=ALU.add),
             reads=[K_("slot")], writes=[K_("slot")])
        P.op("dve", lambda e: e.tensor_tensor(out=slot[:], in0=slot[:], in1=okm[:], op=ALU.mult),
             reads=[K_("slot"), K_("okm")], writes=[K_("slot")])
        P.op("dve", lambda e: e.tensor_scalar(out=slot[:], in0=slot[:], scalar1=float(NSLOT), scalar2=None, op0=ALU.add),
             reads=[K_("slot")], writes=[K_("slot")])
        for k in range(4):
            P.op("dve", lambda e, k=k: e.tensor_scalar(out=sel[:], in0=lg[:], scalar1=mx8[:, k:k + 1], scalar2=None,
                                                       op0=ALU.is_equal), reads=[K_("lg"), K_("mx8")], writes=[K_("sel")])
            P.op("dve", lambda e: e.tensor_tensor(out=tmp32[:], in0=sel[:], in1=slot[:], op=ALU.mult),
                 reads=[K_("sel"), K_("slot")], writes=[K_("tmp32")])
            P.op("dve", lambda e, k=k, t=t: e.reduce_sum(out=slotk[:, t, k:k + 1], in_=tmp32[:], axis=AX.X),
                 reads=[K_("tmp32")], writes=[K_("slotk")])
            P.op("dve", lambda e: e.tensor_tensor(out=tmp32[:], in0=sel[:], in1=gate[:], op=ALU.mult),
                 reads=[K_("sel"), K_("gate")], writes=[K_("tmp32")])
            P.op("dve", lambda e, k=k, t=t: e.reduce_sum(out=gk[:, t, k:k + 1], in_=tmp32[:], axis=AX.X),
                 reads=[K_("tmp32")], writes=[K_("gk")])
        P.op("dve", lambda e, t=t: e.tensor_copy(out=idx[:, t, :], in_=slotk[:, t, :]), reads=[K_("slotk")],
             writes=[K_("idx")])
        for k in range(4):
            P.dma("pool", lambda e, k=k, t=t, h1b=h1b: e.indirect_dma_start(
                out=T["xs"], out_offset=bass.IndirectOffsetOnAxis(ap=idx[:, t, k:k + 1], axis=0), in_=h1b[:],
                in_offset=None), reads=[h1bk, K_("idx")] + XSZ)
    P.barrier()
    es1.close()
    if stop_after < 1:
        return
    cur[0] = es2
    Wgu = [sb(f"Wgu{i}", [128, 8, 2048], BF16) for i in range(2)]
    Wd = [sb(f"Wd{i}", [128, 8, 1024], BF16) for i in range(2)]
    bdb = [sb(f"bdb{i}", [128, 1024], F32) for i in range(2)]
    xsl = [sb(f"xsl{i}", [128, CT, 1024], BF16) for i in range(2)]
    xT = sb("xT", [128, 8, C], BF16)
    gT = sb("gT", [128, 8, C], BF16)
    W1 = Rot([(sb(f"w1_{i}", [128, 512], F32), K_(f"w1_{i}")) for i in range(2)])
    W2 = Rot([(sb(f"w2_{i}", [128, 512], F32), K_(f"w2_{i}")) for i in range(2)])
    W3 = Rot([(sb(f"w3_{i}", [128, 512], F32), K_(f"w3_{i}")) for i in range(2)])
    ysb = Rot([(sb(f"ysb{i}", [128, 1024], F32), K_(f"ysb{i}")) for i in range(2)])
    stg32 = Rot([(sb(f"stg32_{i}", [128, 2048], F32), K_(f"stg32_{i}")) for i in range(4)])

    def load_expert(e_):
        p_ = e_ % 2
        P.dma("sp", lambda e, p_=p_, e_=e_: e.dma_start(out=bdb[p_][:], in_=T["bd"][e_].partition_broadcast(128)),
              writes=[K_(f"bdb{p_}")])
        P.dma("sp", lambda e, p_=p_, e_=e_: e.dma_start(
            out=xsl[p_][:], in_=T["xs"][e_ * C:(e_ + 1) * C, :].rearrange("(a p) f -> p a f", p=128)),
            writes=[K_(f"xsl{p_}")])
        ops = []
        for c in range(8):
            def f(c=c):
                st_, sk = stg32.next()
                P.dma("sp", lambda e, st_=st_, c=c: e.dma_start(out=st_[:], in_=T["wgu"][e_, c * 128:(c + 1) * 128, :]),
                      writes=[sk])
                if c % 2:
                    P.op("act", lambda e, st_=st_, c=c: e.copy(out=Wgu[p_][:, c, :], in_=st_[:]), reads=[sk],
                         writes=[K_(f"Wgu{p_}_{c}")])
                else:
                    P.op("dve", lambda e, st_=st_, c=c: e.tensor_copy(out=Wgu[p_][:, c, :], in_=st_[:]), reads=[sk],
                         writes=[K_(f"Wgu{p_}_{c}")])
            ops.append(f)
        for c2 in range(4):
            def f(c2=c2):
                st_, sk = stg32.next()
                P.dma("sp", lambda e, st_=st_, c2=c2: e.dma_start(
                    out=st_[:].rearrange("p (a n) -> p a n", a=2),
                    in_=T["wd"][e_, c2 * 256:(c2 + 1) * 256, :].rearrange("(a p) n -> p a n", p=128)), writes=[sk])
                if c2 % 2:
                    P.op("act", lambda e, st_=st_, c2=c2: e.copy(out=Wd[p_][:, 2 * c2:2 * c2 + 2, :],
                                                                 in_=st_[:].rearrange("p (a n) -> p a n", a=2)),
                         reads=[sk], writes=[K_(f"Wd{p_}_{c2}")])
                else:
                    P.op("dve", lambda e, st_=st_, c2=c2: e.tensor_copy(out=Wd[p_][:, 2 * c2:2 * c2 + 2, :],
                                                                        in_=st_[:].rearrange("p (a n) -> p a n", a=2)),
                         reads=[sk], writes=[K_(f"Wd{p_}_{c2}")])
            ops.append(f)
        return ops

    pend_ops = load_expert(0)
    for f in pend_ops:
        f()
    pend_ops = []

    def pump(n=1):
        for _ in range(n):
            if pend_ops:
                pend_ops.pop(0)()

    for e_ in range(NE):
        p_ = e_ % 2
        if e_ + 1 < NE:
            pend_ops.extend(load_expert(e_ + 1))
        for a in range(CT):
            for c in range(8):
                P.op("pe", lambda e, p_=p_, a=a, c=c: e.transpose(out=Tb[:, c, :], in_=xsl[p_][:, a, c * 128:(c + 1) * 128],
                                                                  identity=identb[:]),
                     reads=[K_(f"xsl{p_}"), K_("identb")], writes=[TbK])
            P.op("act", lambda e, a=a: e.copy(out=xT[:, :, a * 128:(a + 1) * 128], in_=Tb[:]), reads=[TbK],
                 writes=[K_("xT")])
        for (n0, nn) in RNG:
            for fc in range(8):
                gb_, gbk = Ob.next()
                lb_, lbk = Ob.next()
                for (bank, bk_, col) in ((gb_, gbk, fc * 128), (lb_, lbk, 1024 + fc * 128)):
                    for c in range(8):
                        P.op("pe", lambda e, bank=bank, c=c, col=col, p_=p_, n0=n0, nn=nn: e.matmul(
                            bank[:, 0:nn], lhsT=Wgu[p_][:, c, col:col + 128], rhs=xT[:, c, n0:n0 + nn], start=(c == 0),
                            stop=(c == 7)), reads=[K_(f"Wgu{p_}_{c}"), K_("xT")], writes=[bk_])
                w1, w1k = W1.next()
                w2, w2k = W2.next()
                w3, w3k = W3.next()
                P.op("dve", lambda e, gb_=gb_, w1=w1, fc=fc, e_=e_, nn=nn: e.tensor_scalar(
                    out=w1[:, 0:nn], in0=gb_[:, 0:nn], scalar1=bguT[:, fc, e_:e_ + 1], scalar2=7.0, op0=ALU.add, op1=ALU.min),
                    reads=[gbk, K_("bguT0")], writes=[w1k])
                P.op("act", lambda e, w1=w1, w2=w2, nn=nn: e.activation(out=w2[:, 0:nn], in_=w1[:, 0:nn], func=AF.Sigmoid,
                                                                         scale=1.702), reads=[w1k], writes=[w2k])
                P.op("dve", lambda e, lb_=lb_, w3=w3, fc=fc, e_=e_, nn=nn: e.tensor_scalar(
                    out=w3[:, 0:nn], in0=lb_[:, 0:nn], scalar1=bguT[:, 8 + fc, e_:e_ + 1], scalar2=7.0, op0=ALU.add,
                    op1=ALU.min), reads=[lbk, K_("bguT0")], writes=[w3k])
                P.op("dve", lambda e, w3=w3, nn=nn: e.tensor_scalar(out=w3[:, 0:nn], in0=w3[:, 0:nn], scalar1=-7.0,
                                                                    scalar2=1.0, op0=ALU.max, op1=ALU.add),
                     reads=[w3k], writes=[w3k])
                P.op("dve", lambda e, w1=w1, w2=w2, nn=nn: e.tensor_tensor(out=w1[:, 0:nn], in0=w1[:, 0:nn], in1=w2[:, 0:nn],
                                                                           op=ALU.mult), reads=[w1k, w2k], writes=[w1k])
                P.op("dve", lambda e, w1=w1, w3=w3, fc=fc, n0=n0, nn=nn: e.tensor_tensor(
                    out=gT[:, fc, n0:n0 + nn], in0=w1[:, 0:nn], in1=w3[:, 0:nn], op=ALU.mult), reads=[w1k, w3k],
                    writes=[K_("gT")])
                pump(1)
        for a in range(CT):
            yt, ytk = ysb.next()
            for half in range(2):
                bank, bk_ = Sb.next()
                for fc in range(8):
                    P.op("pe", lambda e, bank=bank, fc=fc, a=a, half=half, p_=p_: e.matmul(
                        bank[:], lhsT=gT[:, fc, a * 128:(a + 1) * 128], rhs=Wd[p_][:, fc, half * 512:(half + 1) * 512],
                        start=(fc == 0), stop=(fc == 7)), reads=[K_("gT"), K_(f"Wd{p_}_{fc // 2}")], writes=[bk_])
                P.op("dve", lambda e, bank=bank, yt=yt, half=half, p_=p_: e.tensor_tensor(
                    out=yt[:, half * 512:(half + 1) * 512], in0=bank[:], in1=bdb[p_][:, half * 512:(half + 1) * 512],
                    op=ALU.add), reads=[bk_, K_(f"bdb{p_}")], writes=[ytk])
            r0 = e_ * C + a * 128
            P.dma("sp", lambda e, yt=yt, r0=r0: e.dma_start(out=T["ys"][r0:r0 + 128, :], in_=yt[:]), reads=[ytk])
            pump(1)
        pump(99)
    P.barrier()
    es2.close()
    if stop_after < 2:
        return
    cur[0] = es3
    g2b = sb("g2b", [128, 1024], F32)
    b2b = sb("b2b", [128, 1024], F32)
    yks = [Rot([(sb(f"yk{k}_{i}", [128, 1024], F32), K_(f"yk{k}_{i}")) for i in range(2)]) for k in range(4)]
    acc = Rot([(sb(f"acc{i}", [128, 1024], F32), K_(f"acc{i}")) for i in range(2)])
    hts = Rot([(sb(f"ht3_{i}", [128, 1024], F32), K_(f"ht3_{i}")) for i in range(2)])
    ld("sp", g2b[:], T["ln2g"].partition_broadcast(128), K_("g2b"))
    ld("sp", b2b[:], T["ln2b"].partition_broadcast(128), K_("b2b"))

    for t in range(NTT):
        ht, hk = hts.next()
        ac, ack = acc.next()
        P.dma("sp", lambda e, ht=ht, t=t: e.dma_start(out=ht[:], in_=T["h1s"][t * 128:(t + 1) * 128, :]),
              writes=[hk])
        for k in range(4):
            yk, ykk = yks[k].next()
            P.dma("pool", lambda e, yk=yk, k=k, t=t: e.indirect_dma_start(
                out=yk[:], out_offset=None, in_=T["ys"],
                in_offset=bass.IndirectOffsetOnAxis(ap=idx[:, t, k:k + 1], axis=0)),
                reads=[K_("idx")], writes=[ykk])
            if k == 0:
                P.op("dve", lambda e, yk=yk, ac=ac, t=t: e.tensor_scalar(out=ac[:], in0=yk[:], scalar1=gk[:, t, 0:1],
                                                                         scalar2=None, op0=ALU.mult),
                     reads=[ykk, K_("gk")], writes=[ack])
                P.op("dve", lambda e, ht=ht, ac=ac: e.scalar_tensor_tensor(out=ac[:], in0=ht[:], scalar=float(ALPHA),
                                                                           in1=ac[:], op0=ALU.mult, op1=ALU.add),
                     reads=[hk, ack], writes=[ack])
            else:
                P.op("dve", lambda e, yk=yk, ac=ac, t=t, k=k: e.scalar_tensor_tensor(
                    out=ac[:], in0=yk[:], scalar=gk[:, t, k:k + 1], in1=ac[:], op0=ALU.mult, op1=ALU.add),
                    reads=[ykk, K_("gk"), ack], writes=[ack])
        layer_norm_tile(ac, ack, g2b, b2b, K_("g2b"), K_("b2b"))
        P.dma("act", lambda e, ac=ac, t=t: e.dma_start(out=T["hout"][t * 128:(t + 1) * 128, :], in_=ac[:]), reads=[ack])
    P.barrier()
    es3.close()


def declare_b_inputs(nc, NTOK, sfx=""):
    T = {}

    def din(name, shape, dt=F32):
        T[name] = nc.dram_tensor(name + sfx, list(shape), dt, kind="ExternalInput").ap()

    din("wout", [D, D])
    din("ln1g", [D])
    din("ln1b", [D])
    din("ln2g", [D])
    din("ln2b", [D])
    din("rw", [D, NE])
    din("rb", [NE])
    din("wgu", [NE, D, 2 * DFF])
    din("bgu", [NE, 2 * DFF])
    din("wd", [NE, DFF, D])
    din("bd", [NE, D])
    return T


def b_inputs(l, inputs):
    g = lambda k: np.ascontiguousarray(np.asarray(inputs[k][l]), dtype=np.float32)
    return dict(wout=g("w_out"), ln1g=g("ln1_g"), ln1b=g("ln1_b"), ln2g=g("ln2_g"), ln2b=g("ln2_b"), rw=g("router_w"),
                rb=g("router_b"), wgu=g("w_gate_up"), bgu=g("b_gate_up"), wd=g("w_down"), bd=g("b_down"))


def build_b(NTOK, C, stop_after=9):
    nc = bass.Bass("TRN2", target_bir_lowering=False)
    T = declare_b_inputs(nc, NTOK)
    declare_consts(nc, T)
    T["mixed"] = nc.dram_tensor("mixed", [NTOK, D], BF16, kind="ExternalInput").ap()
    T["hres"] = nc.dram_tensor("hres", [NTOK, D], F32, kind="ExternalInput").ap()
    T["hout"] = nc.dram_tensor("hout", [NTOK, D], F32, kind="ExternalOutput").ap()
    T["h1s"] = nc.dram_tensor("h1s", [NTOK, D], F32, kind="ExternalOutput").ap()
    T["xs"] = nc.dram_tensor("xs", [NE * C + 128, D], BF16, kind="Internal").ap()
    T["ys"] = nc.dram_tensor("ys", [NE * C + 128, D], F32, kind="Internal").ap()
    with ExitStack() as es:
        P = Prog(nc, es)
        phase_b(nc, P, es, NTOK, C, T, "b_", stop_after)
        P.finish("sp")
        P.emit()
    return nc


S_FULL = 4096
NB = 4
NCORES = 8
_CACHE = {}


def _lam_init(l):
    return 0.8 - 0.6 * math.exp(-0.3 * l)


def build_fused(S, NTOK_B, C, HS=True):
    nc = bass.Bass("TRN2", target_bir_lowering=False)
    split = NTOK_B < S
    TC = {}
    declare_consts(nc, TC)
    TA = [declare_a_inputs(nc, S, True, sfx="_0")]
    for l in range(1, DEPTH):
        T = {}
        for name, shape, dt in (("pos", [S], I32), ("win", [D, NWIN], F32), ("wuq", [256, 1024], F32),
                                ("wukv", [128, 768], F32), ("bf", [4], F32), ("dlam", [128], F32), ("dg", [64], F32),
                                ("qg", [256], F32), ("kvg", [128], F32)):
            T[name] = nc.dram_tensor(f"{name}_{l}", shape, dt, kind="ExternalInput").ap()
        TA.append(T)
    TB = [declare_b_inputs(nc, NTOK_B, sfx=f"_{l}") for l in range(DEPTH)]
    rowidx = nc.dram_tensor("rowidx", [128, NTOK_B // 128], I32, kind="ExternalInput").ap() if split else None
    HS = HS and split
    if HS:
        rowidxg0 = nc.dram_tensor("rowidxg0", [128, NTOK_B // 128], I32, kind="ExternalInput").ap()
        rowidxg1 = nc.dram_tensor("rowidxg1", [128, NTOK_B // 128], I32, kind="ExternalInput").ap()
    hcur = None
    out = nc.dram_tensor("hout", [NTOK_B, D], F32, kind="ExternalOutput").ap()
    with ExitStack() as es:
        P = Prog(nc, es)
        for l in range(DEPTH):
            ta = dict(TA[l])
            ta.update(TC)
            ta["mixed"] = nc.dram_tensor(f"mixed_{l}", [S, D], BF16, kind="Internal").ap()
            if l == 0:
                ta["h0"] = nc.dram_tensor("h0", [S, D], F32, kind="Internal").ap()
                hres = ta["h0"]
            else:
                if split:
                    ta["hin_tile"] = hin_tile
                else:
                    ta["hin"] = hcur
                hres = hcur
            with ExitStack() as esa:
                phase_a(nc, P, esa, S, ta, l == 0, _lam_init(l), f"a{l}_", 9, 2 if HS else 4)
                P.barrier()
            tb = dict(TB[l])
            tb.update(TC)
            tb["mixed"] = ta["mixed"]
            if HS:
                CHM = 1024 if S >= 2048 else S // 2
                mixG = nc.dram_tensor(f"mixG_{l}", [2 * S, D], BF16, kind="Internal").ap()
                for k in range(S // CHM):
                    P.cc(lambda e, k=k, src=ta["mixed"], mixG=mixG: e.collective_compute(
                        "AllGather", ALU.bypass, replica_groups=[[2 * i, 2 * i + 1] for i in range(NCORES // 2)],
                        ins=[src[k * CHM:(k + 1) * CHM, :].opt()], outs=[mixG[k * 2 * CHM:(k + 1) * 2 * CHM, :].opt()]))
                P.barrier()
                tb["mixG"] = mixG
                tb["rowidxg0"] = rowidxg0
                tb["rowidxg1"] = rowidxg1
            tb["hres"] = hres
            if split:
                tb["rowidx"] = rowidx
                if l > 0:
                    tb["hres_local"] = hown_prev
            tb["h1s"] = nc.dram_tensor(f"h1s_{l}", [NTOK_B, D], F32, kind="Internal").ap()
            tb["xs"] = nc.dram_tensor(f"xs_{l}", [NE * C + 128, D], BF16, kind="Internal").ap()
            tb["ys"] = nc.dram_tensor(f"ys_{l}", [NE * C + 128, D], F32, kind="Internal").ap()
            last = (l == DEPTH - 1)
            if last:
                tb["hout"] = out
            else:
                tb["hout"] = nc.dram_tensor(f"hown_{l}", [NTOK_B, D], F32, kind="Internal").ap()
            with ExitStack() as esb:
                phase_b(nc, P, esb, NTOK_B, C, tb, f"b{l}_")
                P.barrier()
            if not last:
                if split:
                    CH = 512 if NTOK_B >= 1024 else NTOK_B // 2
                    NCH = NTOK_B // CH
                    hfull = nc.dram_tensor(f"hfull_{l}", [S, D], F32, kind="Internal").ap()
                    src = tb["hout"]
                    hown_prev = src
                    for k in range(NCH):
                        P.cc(lambda e, src=src, hfull=hfull, k=k: e.collective_compute(
                            "AllGather", ALU.bypass, replica_groups=[[2 * i, 2 * i + 1] for i in range(NCORES // 2)],
                            ins=[src[k * CH:(k + 1) * CH, :].opt()], outs=[hfull[k * 2 * CH:(k + 1) * 2 * CH, :].opt()]))
                    P.barrier()
                    hcur = hfull

                    def hin_tile(t, hfull=hfull, CH=CH):
                        tok = t * 128
                        half, rem = tok // NTOK_B, tok % NTOK_B
                        k, r = rem // CH, rem % CH
                        row = k * 2 * CH + half * CH + r
                        return hfull[row:row + 128, :]
                else:
                    hcur = tb["hout"]
        P.finish("sp")
        P.emit()
    return nc


NTOK_B = 2048
C_B = 512


def kernel(**inputs):
    inputs = {k: np.asarray(v) for k, v in inputs.items()}
    x = inputs["x"].astype(np.float32, copy=False)
    pos = inputs["positions"].astype(np.int32, copy=False)
    consts = host_consts()
    cores = list(range(NCORES))
    if "f" not in _CACHE:
        _CACHE["f"] = build_fused(S_FULL, NTOK_B, C_B, True)
    nc = _CACHE["f"]
    shared = dict(consts)
    for l in range(DEPTH):
        for k_, v in b_inputs(l, inputs).items():
            shared[f"{k_}_{l}"] = v
    per_par = []
    for par in range(2):
        hp = (2 * par, 2 * par + 1, 2 * par, 2 * par + 1)
        d_ = {}
        for l in range(DEPTH):
            am = a_inputs(l, inputs, x[0], pos[0], l == 0, hp)
            for k_ in ("hin", "pos") + tuple(consts.keys()):
                am.pop(k_, None)
            for k_, v in am.items():
                d_[f"{k_}_{l}"] = v
        loc = np.arange(NTOK_B, dtype=np.int64)
        tok = par * NTOK_B + loc
        t2 = lambda a_: np.ascontiguousarray(a_.reshape(NTOK_B // 128, 128).T.astype(np.int32))
        CHM = 1024
        d_["rowidx"] = t2(tok)
        d_["rowidxg0"] = t2(((tok // CHM) * 2 + 0) * CHM + tok % CHM)
        d_["rowidxg1"] = t2(((tok // CHM) * 2 + 1) * CHM + tok % CHM)
        per_par.append(d_)
    maps = []
    for c in cores:
        b, par = c // 2, c % 2
        m = dict(shared)
        m.update(per_par[par])
        m["hin_0"] = np.ascontiguousarray(x[b])
        for l in range(DEPTH):
            m[f"pos_{l}"] = np.ascontiguousarray(pos[b])
        maps.append(m)
    res = run_bass_kernel_spmd(nc, maps, core_ids=cores).results
    return np.stack([np.concatenate([res[2 * b]["hout"], res[2 * b + 1]["hout"]], axis=0) for b in range(NB)]).astype(np.float32)
```

```python
import math
import os
from contextlib import ExitStack

import numpy as np
import ml_dtypes
import concourse.bass as bass
import concourse.mybir as mybir
from concourse.bass_utils import run_bass_kernel_spmd

F32 = mybir.dt.float32
BF16 = mybir.dt.bfloat16
I32 = mybir.dt.int32
AF = mybir.ActivationFunctionType
ALU = mybir.AluOpType
AX = mybir.AxisListType

D = 1024
DEPTH = 2
NE = 32
TOPK = 4
DFF = 1024
LN_EPS = 1e-5
RMS_EPS = 1e-6
ALPHA = (2 * DEPTH) ** 0.25
ROPE_THETA = 10000.0
NWIN = 768 + 1792 + 772 + 640


class Prog:
    ENG = ("pe", "act", "dve", "pool", "sp")

    def __init__(self, nc, es, n_dma_sems=24):
        self.nc = nc
        self._es = es
        self.lists = {e: [] for e in self.ENG}
        self.sem = {e: es.enter_context(nc.semaphore("s_" + e)) for e in self.ENG}
        self.cnt = {e: 0 for e in self.ENG}
        self.dsems = [es.enter_context(nc.semaphore(f"d{i}")) for i in range(n_dma_sems)]
        self.dcnt = [0] * n_dma_sems
        a, b = n_dma_sems // 2, n_dma_sems // 2 + n_dma_sems // 6
        self.dpool = {"sp": list(range(0, a)), "act": list(range(a, b)), "pool": list(range(b, n_dma_sems))}
        self.dnext = {"sp": 0, "act": 0, "pool": 0}
        self.waited = {}
        self.lastw = {}
        self.readers = {}

    def _semh(self, key):
        return self.sem[key[1]] if key[0] == "e" else self.dsems[key[1]]

    def _deps(self, eng, reads, writes, extra=()):
        need = {}

        def add(tok):
            if tok is None:
                return
            k, v = tok
            if need.get(k, 0) < v:
                need[k] = v

        for r in reads:
            add(self.lastw.get(r))
        for w in writes:
            add(self.lastw.get(w))
            for k, v in self.readers.get(w, {}).items():
                add((k, v))
        for t in extra:
            add(t)
        waits = []
        for k, v in need.items():
            if eng == "pe" and k == ("e", "pe"):
                continue
            if self.waited.get((eng, k), 0) >= v:
                continue
            self.waited[(eng, k)] = v
            waits.append((self._semh(k), v))
        return waits

    def _record(self, tok, reads, writes):
        for w in writes:
            self.lastw[w] = tok
            self.readers[w] = {}
        for r in reads:
            d = self.readers.setdefault(r, {})
            if d.get(tok[0], 0) < tok[1]:
                d[tok[0]] = tok[1]

    def op(self, eng, fn, reads=(), writes=()):
        waits = self._deps(eng, reads, writes)
        self.cnt[eng] += 1
        tok = (("e", eng), self.cnt[eng])
        self.lists[eng].append((waits, fn, (self.sem[eng], 1)))
        self._record(tok, reads, writes)
        return tok

    def dma(self, q, fn, reads=(), writes=()):
        pl = self.dpool[q]
        i = pl[self.dnext[q]]
        self.dnext[q] = (self.dnext[q] + 1) % len(pl)
        prev = (("d", i), self.dcnt[i]) if self.dcnt[i] else None
        waits = self._deps(q, reads, writes, extra=(prev,) if prev else ())
        self.dcnt[i] += 16
        tok = (("d", i), self.dcnt[i])
        self.lists[q].append((waits, fn, (self.dsems[i], 16)))
        self._record(tok, reads, writes)
        return tok

    def cc(self, fn, reads=(), writes=(), inc=1):
        if not hasattr(self, "ccsem"):
            self.ccsem = self._es.enter_context(self.nc.semaphore("ccsem"))
            self.dsems.append(self.ccsem)
            self.dcnt.append(0)
        i = len(self.dsems) - 1
        prev = (("d", i), self.dcnt[i]) if self.dcnt[i] else None
        waits = self._deps("pool", reads, writes, extra=(prev,) if prev else ())
        self.dcnt[i] += inc
        tok = (("d", i), self.dcnt[i])
        self.lists["pool"].append((waits, fn, (self.dsems[i], inc)))
        self._record(tok, reads, writes)
        return tok

    def finish(self, eng="sp"):
        waits = []
        for i, c in enumerate(self.dcnt):
            if c and self.waited.get((eng, ("d", i)), 0) < c:
                waits.append((self.dsems[i], c))
        for e in self.ENG:
            if e != eng and self.cnt[e] and self.waited.get((eng, ("e", e)), 0) < self.cnt[e]:
                waits.append((self.sem[e], self.cnt[e]))
        self.lists[eng].append((waits, None, None))

    def barrier(self):
        for eng in self.ENG:
            waits = []
            for i, c in enumerate(self.dcnt):
                if c and self.waited.get((eng, ("d", i)), 0) < c:
                    waits.append((self.dsems[i], c))
                    self.waited[(eng, ("d", i))] = c
            for e in self.ENG:
                if self.cnt[e] and self.waited.get((eng, ("e", e)), 0) < self.cnt[e]:
                    waits.append((self.sem[e], self.cnt[e]))
                    self.waited[(eng, ("e", e))] = self.cnt[e]
            self.lists[eng].append((waits, None, None))

    def emit(self):
        nc = self.nc
        lists = self.lists

        def run(engobj, items):
            for waits, fn, inc in items:
                for s, v in waits:
                    engobj.wait_ge(s, v)
                if fn is None:
                    continue
                ins = fn(engobj)
                if inc is not None:
                    ins.then_inc(inc[0], inc[1])

        with nc.Block() as block:
            @block.tensor
            def _(e):
                run(e, lists["pe"])

            @block.scalar
            def _(e):
                run(e, lists["act"])

            @block.vector
            def _(e):
                run(e, lists["dve"])

            @block.gpsimd
            def _(e):
                run(e, lists["pool"])

            @block.sync
            def _(e):
                run(e, lists["sp"])


class Rot:
    def __init__(self, items):
        self.items = items
        self.i = 0

    def next(self):
        it = self.items[self.i]
        self.i = (self.i + 1) % len(self.items)
        return it


def host_consts():
    k = np.arange(128)[:, None]
    q = np.arange(128)[None, :]
    c = {}
    c["ident"] = np.eye(128, dtype=np.float32)
    c["maskd"] = (q >= k).astype(np.float32)
    c["masks"] = (q > k).astype(np.float32)
    c["negtri"] = np.where(k >= q, -8.0, 0.0).astype(np.float32)
    c["triincl"] = (k <= q).astype(np.float32)
    c["ones"] = np.ones((128, 128), np.float32)
    e0 = np.zeros((128, 128), np.float32)
    e0[0, :] = 1.0
    c["e0"] = e0
    p = np.arange(128)
    i = p % 16
    freq = (ROPE_THETA ** (-(2.0 * i) / 32.0)) / (2.0 * math.pi)
    freq = np.where(p < 96, freq, 0.0)
    sgn = np.where((p % 32) < 16, -1.0, 1.0)
    c["ropef"] = np.stack([freq, sgn], axis=1).astype(np.float32)
    c["iota_e"] = np.tile(np.arange(NE, dtype=np.float32)[None, :], (128, 1))
    c["tristrict"] = (k < q).astype(np.float32)
    return c


def prep_w_in(w, hp=(0, 1, 2, 3)):
    out = np.zeros((D, NWIN), np.float32)
    o = 0
    for base in (0, 256, 512):
        for h in range(4):
            out[:, o + h * 64:o + h * 64 + 64] = w[:, base + hp[h] * 64:base + hp[h] * 64 + 64]
        o += 256
    for src in (768, 1024):
        for rot in (0, 1):
            for u in range(8):
                ug = 2 * hp[u // 2] + (u % 2)
                dst = o + (u // 3) * 128 + (u % 3) * 32
                s0 = src + ug * 32
                if rot == 0:
                    out[:, dst:dst + 32] = w[:, s0:s0 + 32]
                else:
                    out[:, dst:dst + 16] = w[:, s0 + 16:s0 + 32]
                    out[:, dst + 16:dst + 32] = w[:, s0:s0 + 16]
            o += 384
    for h in range(4):
        out[:, o + h * 64:o + h * 64 + 64] = w[:, 1280 + hp[h] * 64:1280 + hp[h] * 64 + 64]
    o += 256
    for base in (1536, 1792, 2048):
        for h in range(4):
            out[:, o + h * 64:o + h * 64 + 64] = w[:, base + hp[h] * 64:base + hp[h] * 64 + 64]
        o += 256
    for h in range(4):
        out[:, o + h] = w[:, 2304 + hp[h]]
    o += 4
    out[:, o:o + 384] = w[:, 2308:2692]
    o += 384
    out[:, o + 64:o + 96] = w[:, 2692:2724]
    o += 128
    out[:, o + 64:o + 80] = w[:, 2692 + 16:2724]
    out[:, o + 80:o + 96] = w[:, 2692:2692 + 16]
    o += 128
    assert o == NWIN
    return out


def prep_w_uq(w, hp=(0, 1, 2, 3)):
    out = np.zeros((256, 1024), np.float32)
    for h in range(4):
        g = hp[h]
        a = h * 256
        out[:, a:a + 96] = w[:, g * 96:g * 96 + 96]
        b = a + 128
        out[:, b + 64:b + 80] = w[:, g * 96 + 80:g * 96 + 96]
        out[:, b + 80:b + 96] = w[:, g * 96 + 64:g * 96 + 80]
    return out


def prep_w_ukv(w, hp=(0, 1, 2, 3)):
    out = np.zeros((128, 768), np.float32)
    for h in range(4):
        g = hp[h]
        out[:, h * 128:h * 128 + 64] = w[:, g * 128:g * 128 + 64]
        out[:, 512 + h * 64:512 + h * 64 + 64] = w[:, g * 128 + 64:g * 128 + 128]
    return out


def phase_a(nc, P, es, S, T, pre_ln, lam_init, tag, stop_after=9, NHP=4):
    NT = S // 128
    cur = [es]
    NJ = S // 512

    def sb(name, shape, dt):
        return cur[0].enter_context(nc.sbuf_tensor(tag + name, shape, dt))

    def ps(name, shape, dt):
        return es.enter_context(nc.psum_tensor(tag + name, shape, dt))

    hT = sb("hT", [128, 8, S], BF16)
    identb = sb("identb", [128, 128], BF16)
    _ob = [(ps(f"O{i}", [128, 512], F32), f"O{i}") for i in range(4)]
    Ob = Rot(_ob)
    ObSB = Rot(_ob[0:3])
    ObA = Rot(_ob[0:2])
    Pb = Rot(_ob[2:4])
    pools = {"S": None, "O": None}
    Tb = ps("Tb", [128, 8, 128], BF16)

    def ld(q, dst, src, key):
        P.dma(q, lambda e: e.dma_start(out=dst, in_=src), writes=[key])

    ld("pool", identb[:], T["ident"], "identb")
    es_h = ExitStack()
    cur[0] = es_h
    hld = Rot([(sb(f"hld{i}", [128, 1024], F32), f"hld{i}") for i in range(3)])
    hbf = Rot([(sb(f"hbf{i}", [128, 1024], BF16), f"hbf{i}") for i in range(3)])
    st6 = sb("st6", [128, 2, 6], F32)
    mv = sb("mv", [128, 2], F32)
    if pre_ln:
        lngb = sb("lngb", [128, 1024], F32)
        lnbb = sb("lnbb", [128, 1024], F32)
    if pre_ln:
        ld("sp", lngb[:], T["lng"].partition_broadcast(128), "lngb")
        ld("sp", lnbb[:], T["lnb"].partition_broadcast(128), "lnbb")
    def layer_norm_tile(src, srckey, dst, dstkey, gb, bb, gk, bk_):
        for c in range(2):
            P.op("dve", lambda e, c=c: e.bn_stats(out=st6[:, c, :], in_=src[:, c * 512:(c + 1) * 512]),
                 reads=[srckey], writes=["st6"])
        P.op("dve", lambda e: e.bn_aggr(out=mv[:], in_=st6[:].rearrange("p a b -> p (a b)")), reads=["st6"],
             writes=["mv"])
        P.op("act", lambda e: e.activation(out=mv[:, 1:2], in_=mv[:, 1:2], func=AF.Ln, bias=float(LN_EPS)),
             reads=["mv"], writes=["mv"])
        P.op("act", lambda e: e.activation(out=mv[:, 1:2], in_=mv[:, 1:2], func=AF.Exp, scale=-0.5),
             reads=["mv"], writes=["mv"])
        P.op("dve", lambda e: e.scalar_tensor_tensor(out=dst, in0=src, scalar=mv[:, 0:1], in1=gb, op0=ALU.subtract,
                                                     op1=ALU.mult), reads=[srckey, "mv", gk], writes=[dstkey])
        P.op("dve", lambda e: e.scalar_tensor_tensor(out=dst, in0=dst, scalar=mv[:, 1:2], in1=bb, op0=ALU.mult,
                                                     op1=ALU.add), reads=[dstkey, "mv", bk_], writes=[dstkey])

    for t in range(NT):
        ht, hk = hld.next()
        hb, hbk = hbf.next()
        src_t = T["hin_tile"](t) if "hin_tile" in T else T["hin"][t * 128:(t + 1) * 128, :]
        P.dma("sp", lambda e, ht=ht, src_t=src_t: e.dma_start(out=ht[:], in_=src_t), writes=[hk])
        if pre_ln:
            layer_norm_tile(ht[:], hk, ht[:], hk, lngb[:], lnbb[:], "lngb", "lnbb")
            P.dma("act", lambda e, ht=ht, t=t: e.dma_start(out=T["h0"][t * 128:(t + 1) * 128, :], in_=ht[:]), reads=[hk])
        P.op("act", lambda e, ht=ht, hb=hb: e.copy(out=hb[:], in_=ht[:]), reads=[hk], writes=[hbk])
        for c in range(8):
            P.op("pe", lambda e, hb=hb, c=c: e.transpose(out=Tb[:, c, :], in_=hb[:, c * 128:(c + 1) * 128],
                                                          identity=identb[:]), reads=[hbk, "identb"], writes=["Tb"])
        P.op("dve", lambda e, t=t: e.tensor_copy(out=hT[:, :, t * 128:(t + 1) * 128], in_=Tb[:]), reads=["Tb"],
             writes=[("hT", t // 4)])

    P.barrier()
    es_h.close()
    cur[0] = es
    KT = sb("KT", [128, 4, S], BF16)
    V = sb("V", [128, NT, 4, 65], BF16)
    QT = [sb(f"QT{i}", [128, 4, 512], BF16) for i in range(2)]
    Wm = sb("Wm", [128, 8, 1792], BF16)
    Wuq = sb("Wuq", [128, 2, 1024], BF16)
    Wukv = sb("Wukv", [128, 768], BF16)
    mD = sb("mD", [128, 128], BF16)
    mS = sb("mS", [128, 128], BF16)
    negtri = sb("negtri", [128, 128], BF16)
    onesb = sb("onesb", [128, 128], BF16)
    trib = sb("trib", [128, 128], BF16)
    e0b = sb("e0b", [128, 128], BF16)
    lsp = sb("lsp", [128, 4, 3, 4], BF16)
    lr1 = sb("lr1", [128, 4, 4], F32)
    lr2 = sb("lr2", [128, 4, 4], F32)
    ropef = sb("ropef", [128, 2], F32)
    cosT = sb("cosT", [128, 512], F32)
    sinT = sb("sinT", [128, 512], F32)
    posi = sb("posi", [128, 512], I32)
    posf = sb("posf", [128, 512], F32)
    rt1 = sb("rt1", [128, 512], F32)
    rt2 = sb("rt2", [128, 512], F32)
    rti = sb("rti", [128, 512], I32)
    PTs = Rot([(sb(f"PT{i}", [128, 512], BF16), f"PT{i}") for i in range(3)])
    E32 = Rot([(sb(f"E32{i}", [128, 512], F32), f"E32{i}") for i in range(2)])
    SPB = Rot([(sb(f"SPB{i}", [128, 512], BF16), f"SPB{i}") for i in range(4)])
    osb = sb("osb", [128, 4, 64], F32)
    tmpo = sb("tmpo", [128, 4, 64], F32)
    tmpo2 = sb("tmpo2", [128, 4, 64], F32)
    fbuf = sb("fbuf", [128, 4], F32)
    rinv = sb("rinv", [128, 8], F32)
    stage = Rot([(sb(f"stage{i}", [128, 4, 256], BF16), f"stage{i}") for i in range(2)])
    bfb = sb("bfb", [128, 4], F32)
    dlb = sb("dlb", [128, 128], F32)
    dgb = sb("dgb", [128, 4, 64], F32)
    qgb = sb("qgb", [128, 256], F32)
    kvgb = sb("kvgb", [128, 128], F32)
    lam = sb("lam", [128, 4], F32)
    xg = sb("xg", [128, 4, 4], F32)
    lgt = sb("lgt", [128, 4, 4], F32)
    G = sb("G", [128, NT, 4], F32)
    Gc = sb("Gc", [128, 4], F32)
    Gmid = sb("Gmid", [128, 4], F32)
    FB = sb("FB", [128, 2, 4, NT], F32)
    cqs = sb("cqs", [128, 4, 384], BF16)
    cqT = sb("cqT", [128, 3, 512], BF16)
    krope = sb("krope", [128, 512], BF16)
    qtmp = sb("qtmp", [128, 512], BF16)
    ssq = sb("ssq", [128, 8], F32)
    junk = sb("junk", [128, 256], F32)

    Sb = Rot([(ps(f"S{i}", [128, 512], F32), f"S{i}") for i in range(3)])
    pools["S"], pools["O"] = Sb, Ob
    ld("pool", mD[:], T["maskd"], "mD")
    ld("pool", mS[:], T["masks"], "mS")
    ld("pool", negtri[:], T["negtri"], "negtri")
    ld("pool", onesb[:], T["ones"], "onesb")
    ld("pool", trib[:], T["triincl"], "trib")
    ld("pool", e0b[:], T["e0"], "e0b")
    ld("sp", ropef[:], T["ropef"], "ropef")
    ld("sp", bfb[:], T["bf"].partition_broadcast(128), "bfb")
    ld("sp", dlb[:], T["dlam"].partition_broadcast(128), "dlb")
    for q_ in range(4):
        ld("sp", dgb[:, q_, :], T["dg"].partition_broadcast(128), "dgb")
    ld("sp", qgb[:], T["qg"].partition_broadcast(128), "qgb")
    ld("sp", kvgb[:], T["kvg"].partition_broadcast(128), "kvgb")
    ld("pool", Wuq[:], T["wuq"].rearrange("(c p) n -> p c n", p=128), "Wuq")
    ld("pool", Wukv[:], T["wukv"], "Wukv")
    P.op("pool", lambda e: e.memset(V[:], 1.0), writes=["Vall"])
    P.op("dve", lambda e: e.tensor_tensor(out=junk[:, 0:32], in0=dlb[:, 0:32], in1=dlb[:, 32:64], op=ALU.mult),
         reads=["dlb"], writes=["junk"])
    P.op("dve", lambda e: e.reduce_sum(out=lam[:, 1:2], in_=junk[:, 0:32], axis=AX.X), reads=["junk"], writes=["lam1"])
    P.op("dve", lambda e: e.tensor_tensor(out=junk[:, 32:64], in0=dlb[:, 64:96], in1=dlb[:, 96:128], op=ALU.mult),
         reads=["dlb"], writes=["junk"])
    P.op("dve", lambda e: e.reduce_sum(out=lam[:, 2:3], in_=junk[:, 32:64], axis=AX.X), reads=["junk"], writes=["lam2"])
    P.op("act", lambda e: e.activation(out=lam[:, 1:3], in_=lam[:, 1:3], func=AF.Exp), reads=["lam1", "lam2"],
         writes=["lam1", "lam2"])
    P.op("dve", lambda e: e.scalar_tensor_tensor(out=lam[:, 0:1], in0=lam[:, 2:3], scalar=-float(lam_init),
                                                 in1=lam[:, 1:2], op0=ALU.add, op1=ALU.subtract),
         reads=["lam1", "lam2"], writes=["lam0"])
    P.op("dve", lambda e: e.tensor_scalar(out=dgb[:], in0=dgb[:], scalar1=float(1.0 - lam_init), scalar2=None,
                                          op0=ALU.mult), reads=["dgb"], writes=["dgb"])

    def proj_fm(j, wcol, dst_ap, dstkey, post=None):
        bank, bk = pools["S"].next()
        for kc in range(8):
            P.op("pe", lambda e, kc=kc, bank=bank: e.matmul(bank[:], lhsT=Wm[:, kc, wcol:wcol + 128],
                                                             rhs=hT[:, kc, j * 512:(j + 1) * 512],
                                                             start=(kc == 0), stop=(kc == 7)),
                 reads=["Wm", ("hT", j)], writes=[bk])
        if post is None:
            P.op("dve", lambda e, bank=bank: e.tensor_copy(out=dst_ap, in_=bank[:]), reads=[bk], writes=[dstkey])
        return bank, bk

    def rope_tables(j):
        P.dma("sp", lambda e: e.dma_start(out=posi[:], in_=T["pos"][j * 512:(j + 1) * 512].partition_broadcast(128)),
              writes=["posi"])
        P.op("dve", lambda e: e.tensor_copy(out=posf[:], in_=posi[:]), reads=["posi"], writes=["posf"])
        for tab, off, key in ((sinT, 0.0, "sinT"), (cosT, 0.25, "cosT")):
            P.op("dve", lambda e, off=off: e.tensor_scalar(out=rt1[:], in0=posf[:], scalar1=ropef[:, 0:1], scalar2=off,
                                                           op0=ALU.mult, op1=ALU.add), reads=["posf", "ropef"],
                 writes=["rt1"])
            P.op("dve", lambda e: e.tensor_copy(out=rti[:], in_=rt1[:]), reads=["rt1"], writes=["rti"])
            P.op("dve", lambda e: e.tensor_copy(out=rt2[:], in_=rti[:]), reads=["rti"], writes=["rt2"])
            P.op("dve", lambda e: e.tensor_tensor(out=rt1[:], in0=rt1[:], in1=rt2[:], op=ALU.subtract),
                 reads=["rt1", "rt2"], writes=["rt1"])
            P.op("dve", lambda e: e.tensor_scalar(out=rt2[:], in0=rt1[:], scalar1=0.5, scalar2=None, op0=ALU.is_gt),
                 reads=["rt1"], writes=["rt2"])
            P.op("dve", lambda e: e.tensor_tensor(out=rt1[:], in0=rt1[:], in1=rt2[:], op=ALU.subtract),
                 reads=["rt1", "rt2"], writes=["rt1"])
            P.op("dve", lambda e: e.tensor_scalar(out=rt2[:], in0=rt1[:], scalar1=-0.5, scalar2=None, op0=ALU.is_lt),
                 reads=["rt1"], writes=["rt2"])
            P.op("dve", lambda e: e.tensor_tensor(out=rt1[:], in0=rt1[:], in1=rt2[:], op=ALU.add),
                 reads=["rt1", "rt2"], writes=["rt1"])
            P.op("act", lambda e, tab=tab: e.activation(out=tab[:], in_=rt1[:], func=AF.Sin, scale=2.0 * math.pi),
                 reads=["rt1"], writes=[key])
        P.op("dve", lambda e: e.tensor_scalar(out=sinT[:], in0=sinT[:], scalar1=ropef[:, 1:2], scalar2=None,
                                              op0=ALU.mult), reads=["sinT", "ropef"], writes=["sinT"])

    def rope_evac(bA, kA, bB, kB, dst_ap, dstkey, p0, p1):
        P.op("dve", lambda e: e.tensor_tensor(out=rt1[p0:p1, :], in0=bA[p0:p1, :], in1=cosT[p0:p1, :], op=ALU.mult),
             reads=[kA, "cosT"], writes=["rt1"])
        P.op("dve", lambda e: e.tensor_tensor(out=rt2[p0:p1, :], in0=bB[p0:p1, :], in1=sinT[p0:p1, :], op=ALU.mult),
             reads=[kB, "sinT"], writes=["rt2"])
        P.op("pool", lambda e: e.tensor_tensor(out=dst_ap, in0=rt1[p0:p1, :], in1=rt2[p0:p1, :], op=ALU.add),
             reads=["rt1", "rt2"], writes=[dstkey])

    def zero_qt():
        for i_ in range(2):
            P.op("dve", lambda e, i_=i_: e.memset(QT[i_][:], 0.0), writes=[("QT", i_)])

    def proj_q_padded(j, wcol, qt, qkey, c):
        bank, bk = proj_fm(j, wcol, None, None, post=True)
        for hh in range(2):
            P.op("dve", lambda e, bank=bank, hh=hh: e.tensor_copy(out=qt[64 * hh:64 * hh + 64, 2 * c + hh, :],
                                                                  in_=bank[64 * hh:64 * hh + 64, :]),
                 reads=[bk], writes=[qkey])

    def proj_v(j, wcol, ncols, extra=None):
        for t in range(4):
            tt = 4 * j + t
            bank, bk = pools["O"].next()
            for kc in range(8):
                P.op("pe", lambda e, kc=kc, bank=bank, tt=tt: e.matmul(bank[:, 0:ncols],
                                                                       lhsT=hT[:, kc, tt * 128:(tt + 1) * 128],
                                                                       rhs=Wm[:, kc, wcol:wcol + ncols],
                                                                       start=(kc == 0), stop=(kc == 7)),
                     reads=["Wm", ("hT", j)], writes=[bk])
            P.op("dve", lambda e, bank=bank, tt=tt: e.tensor_copy(out=V[:, tt, :, 0:64],
                                                                  in_=bank[:, 0:256].rearrange("p (a b) -> p a b", b=64)),
                 reads=[bk, "Vall"], writes=[("V", j)])
            if extra is not None:
                extra(t, bank, bk)

    def pv_and_norm(h, j, ob, obk):
        pass

    tile_hook = [None]

    def run_pipelined(pre, att):
        def step(gen):
            pools["S"], pools["O"] = Pb, Pb
            try:
                return next(gen, "done")
            finally:
                pools["S"], pools["O"] = Sb, Ob

        g0 = pre(0)
        while step(g0) != "done":
            pass
        for j in range(NJ):
            nxt = pre(j + 1) if j + 1 < NJ else None
            if nxt is not None:
                tile_hook[0] = lambda nxt=nxt: step(nxt)
            att(j)
            tile_hook[0] = None
            if nxt is not None:
                while step(nxt) != "done":
                    pass

    def softmax_head(j, qt, qkey, chunk, base, d, h, scale, bias_fn, ob, obk, qchunk=None, biaskey="FB"):
        qchunk = chunk if qchunk is None else qchunk
        nkt = 4 * (j + 1)
        first = True
        LOOK = 2
        pend = {}

        def qk(kt):
            sbk_t, sbk = Sb.next()
            P.op("pe", lambda e, sbk_t=sbk_t, kt=kt: e.matmul(sbk_t[:], lhsT=KT[base:base + d, chunk, kt * 128:(kt + 1) * 128],
                                                              rhs=qt[base:base + d, qchunk, :], start=True, stop=True),
                 reads=[("KT", chunk, kt // 4), qkey], writes=[sbk])
            pend[kt] = (sbk_t, sbk)

        for kt in range(min(LOOK, nkt)):
            qk(kt)
        for kt in range(nkt):
            sbk_t, sbk = pend.pop(kt)
            pt, ptk = PTs.next()
            if bias_fn is None:
                P.op("act", lambda e, sbk_t=sbk_t, pt=pt: e.activation(out=pt[:], in_=sbk_t[:], func=AF.Exp, scale=scale),
                     reads=[sbk], writes=[ptk])
            else:
                bap = bias_fn(kt)
                P.op("act", lambda e, sbk_t=sbk_t, pt=pt, bap=bap: e.activation(out=pt[:], in_=sbk_t[:], func=AF.Exp,
                                                                                 scale=scale, bias=bap),
                     reads=[sbk, biaskey], writes=[ptk])
            i = kt - 4 * j
            if i >= 0:
                P.op("pool", lambda e, pt=pt, i=i: e.tensor_tensor(out=pt[:, i * 128:(i + 1) * 128],
                                                                   in0=pt[:, i * 128:(i + 1) * 128], in1=mD[:],
                                                                   op=ALU.mult), reads=[ptk, "mD"], writes=[ptk])
            if kt + LOOK < nkt:
                qk(kt + LOOK)
            for qs in range(max(i, 0), 4):
                last = (kt == 4 * j + qs)
                P.op("pe", lambda e, pt=pt, qs=qs, kt=kt, first=first, last=last: e.matmul(
                    ob[:, qs * 65:(qs + 1) * 65], lhsT=pt[:, qs * 128:(qs + 1) * 128], rhs=V[:, kt, h, :],
                    start=first, stop=last, skip_group_check=True), reads=[ptk, ("V", kt // 4)], writes=[obk])
                first = False
            if tile_hook[0] is not None:
                tile_hook[0]()

    def normalize(ob, obk, dst_ap, dstkey, rcol):
        o3 = ob[:, 0:260].rearrange("p (a b) -> p a b", b=65)
        P.op("dve", lambda e: e.reciprocal(out=rinv[:, rcol:rcol + 4], in_=o3[:, :, 64]), reads=[obk], writes=[("rinv", rcol)])
        P.op("dve", lambda e: e.tensor_tensor(out=dst_ap, in0=o3[:, :, 0:64],
                                              in1=rinv[:, rcol:rcol + 4].unsqueeze(2).to_broadcast([128, 4, 64]),
                                              op=ALU.mult), reads=[obk, ("rinv", rcol)], writes=[dstkey])

    def stage_out(j, m, stg, stk):
        dst = T["mixed"][j * 512:(j + 1) * 512, m * 256:(m + 1) * 256].rearrange("(a p) f -> p a f", p=128)
        P.dma("sp", lambda e: e.dma_start(out=dst, in_=stg[:]), reads=[stk])

    def load_wm(c0, n):
        P.dma("pool", lambda e: e.dma_start(out=Wm[:, :, 0:n],
                                            in_=T["win"][:, c0:c0 + n].rearrange("(c p) n -> p c n", p=128)),
              writes=["Wm"])

    def sb_head(j, qt, qkey, chunk, base, h, stg, stk):
        scale = 0.125
        kts = list(range(4 * (j + 1) - 1, -1, -1))
        n = len(kts)
        rb, rbk = _ob[3]
        P.op("dve", lambda e: e.memset(osb[:], 0.0), writes=["osb"])
        P.op("dve", lambda e: e.memset(fbuf[:], 1.0), writes=["fbuf"])
        st = {}
        rfirst = [True]

        def S1(kt):
            i = kt - 4 * j
            zb, zk = Sb.next()
            e32, ek = E32.next()
            spb, spk = SPB.next()
            P.op("pe", lambda e, zb=zb, kt=kt: e.matmul(zb[:], lhsT=KT[:, chunk, kt * 128:(kt + 1) * 128],
                                                        rhs=qt[:, h, :], start=True, stop=True),
                 reads=[("KT", chunk, kt // 4), qkey], writes=[zk])
            P.op("act", lambda e, zb=zb, e32=e32: e.activation(out=e32[:], in_=zb[:], func=AF.Exp, scale=scale),
                 reads=[zk], writes=[ek])
            P.op("act", lambda e, e32=e32, spb=spb: e.activation(out=spb[:], in_=e32[:], func=AF.Ln, bias=1.0),
                 reads=[ek], writes=[spk])
            if i >= 0:
                if i > 0:
                    P.op("pool", lambda e, spb=spb, i=i: e.memset(spb[:, 0:i * 128], 0.0), writes=[spk])
                P.op("pool", lambda e, spb=spb, i=i: e.tensor_tensor(out=spb[:, i * 128:(i + 1) * 128],
                                                                     in0=spb[:, i * 128:(i + 1) * 128], in1=mS[:],
                                                                     op=ALU.mult), reads=[spk, "mS"], writes=[spk])
            st[kt] = dict(zb=zb, zk=zk, spb=spb, spk=spk, i=i)

        def S2(kt):
            d_ = st[kt]
            zb, zk, spb, spk, i = d_["zb"], d_["zk"], d_["spb"], d_["spk"], d_["i"]
            wb, wk = PTs.next()
            P.op("pe", lambda e, zb=zb, spb=spb: e.matmul(zb[:], lhsT=negtri[:], rhs=spb[:], start=False, stop=True,
                                                          skip_group_check=True), reads=[spk, "negtri"], writes=[zk])
            P.op("act", lambda e, zb=zb, wb=wb: e.activation(out=wb[:], in_=zb[:], func=AF.Exp, scale=scale),
                 reads=[zk], writes=[wk])
            if i >= 0:
                P.op("pool", lambda e, wb=wb, i=i: e.tensor_tensor(out=wb[:, i * 128:(i + 1) * 128],
                                                                   in0=wb[:, i * 128:(i + 1) * 128], in1=mS[:],
                                                                   op=ALU.mult), reads=[wk, "mS"], writes=[wk])
            d_["wb"], d_["wk"] = wb, wk

        def S3(kt):
            d_ = st.pop(kt)
            spb, spk, i, wb, wk = d_["spb"], d_["spk"], d_["i"], d_["wb"], d_["wk"]
            q0 = max(i, 0)
            ol, olk = ObSB.next()
            pfirst = True
            for qs in range(q0, 4):
                P.op("pe", lambda e, wb=wb, qs=qs, kt=kt, ol=ol, pfirst=pfirst: e.matmul(
                    ol[:, qs * 64:(qs + 1) * 64], lhsT=wb[:, qs * 128:(qs + 1) * 128], rhs=V[:, kt, h, 0:64],
                    start=pfirst, stop=True, skip_group_check=True), reads=[wk, ("V", kt // 4)], writes=[olk])
                pfirst = False
            fq0 = q0 + 1 if i >= 0 else 0
            if fq0 < 4 and not rfirst[0]:
                P.op("act", lambda e, fq0=fq0: e.activation(out=fbuf[:, fq0:4], in_=rb[:, fq0:4], func=AF.Exp, scale=-1.0),
                     reads=[rbk], writes=["fbuf"])
            o3 = ol[:, 0:256].rearrange("p (a b) -> p a b", b=64)
            P.op("dve", lambda e, o3=o3, q0=q0: e.tensor_tensor(
                out=tmpo[:, q0:4, :], in0=o3[:, q0:4, :],
                in1=fbuf[:, q0:4].unsqueeze(2).to_broadcast([128, 4 - q0, 64]), op=ALU.mult),
                 reads=[olk, "fbuf"], writes=["tmpo"])
            P.op("pool", lambda e, q0=q0: e.tensor_tensor(out=osb[:, q0:4, :], in0=osb[:, q0:4, :], in1=tmpo[:, q0:4, :],
                                                          op=ALU.add), reads=["tmpo", "osb"], writes=["osb"])
            for qs in range(q0, 4):
                P.op("pe", lambda e, spb=spb, qs=qs, rf=rfirst[0]: e.matmul(
                    rb[:, qs:qs + 1], lhsT=spb[:, qs * 128:(qs + 1) * 128], rhs=onesb[:, 0:1], start=rf, stop=True,
                    skip_group_check=True), reads=[spk, "onesb", "fbuf"], writes=[rbk])
                rfirst[0] = False

        for it in range(n + 2):
            if it < n:
                S1(kts[it])
            if 0 <= it - 1 < n:
                S2(kts[it - 1])
            if 0 <= it - 2 < n:
                S3(kts[it - 2])
        P.op("act", lambda e: e.copy(out=stg[:, :, h * 64:(h + 1) * 64], in_=osb[:]), reads=["osb"], writes=[stk])

    if stop_after < 1:
        return
    SKIP = os.environ.get("SKIPMIX", "")
    for stg_, stk_ in stage.items:
        P.op("pool", lambda e, stg_=stg_: e.memset(stg_[:], 0.0), writes=[stk_])
    P.op("dve", lambda e: e.memset(KT[:], 0.0), writes=[("KT", c_, j_) for c_ in range(4) for j_ in range(NJ)])
    zero_qt()
    load_wm(0, 768)
    for j in range(NJ if "0" not in SKIP else 0):
        qt = QT[j % 2]
        qkey = ("QT", j % 2)
        for c in range(max(1, NHP // 2)):
            proj_q_padded(j, c * 128, qt, qkey, c)
            proj_fm(j, 256 + c * 128, KT[:, c, j * 512:(j + 1) * 512], ("KT", c, j))
        proj_v(j, 512, 256)
        stg, stk = stage.next()
        for h in range(NHP):
            sb_head(j, qt, qkey, h // 2, 64 * (h % 2), h, stg, stk)
        stage_out(j, 0, stg, stk)

    if stop_after < 2:
        return
    zero_qt()
    load_wm(768, 1792)
    sc_d = 1.0 / math.sqrt(32.0)
    for j in range(NJ if "1" not in SKIP else 0):
        qt = QT[j % 2]
        qkey = ("QT", j % 2)
        rope_tables(j)
        for c in range((2 * NHP + 2) // 3):
            bA, kA = proj_fm(j, c * 128, None, None, post=True)
            bB, kB = proj_fm(j, 384 + c * 128, None, None, post=True)
            rope_evac(bA, kA, bB, kB, qtmp[0:96, :], "qtmp", 0, 96)
            for u_ in range(3 * c, min(3 * c + 3, 2 * NHP)):
                sl = 32 * (u_ % 3)
                P.op("dve", lambda e, u_=u_, sl=sl, qt=qt: e.tensor_copy(out=qt[sl:sl + 32, u_, :], in_=qtmp[sl:sl + 32, :]),
                     reads=["qtmp"], writes=[qkey])
            bA, kA = proj_fm(j, 768 + c * 128, None, None, post=True)
            bB, kB = proj_fm(j, 1152 + c * 128, None, None, post=True)
            rope_evac(bA, kA, bB, kB, KT[0:96, c, j * 512:(j + 1) * 512], ("KT", c, j), 0, 96)
        proj_v(j, 1536, 256)
        stg, stk = stage.next()
        for h in range(NHP):
            o1, o1k = Ob.next()
            o2, o2k = Ob.next()
            u1, u2 = 2 * h, 2 * h + 1
            softmax_head(j, qt, qkey, u1 // 3, 0, 128, h, sc_d, None, o1, o1k, qchunk=u1)
            softmax_head(j, qt, qkey, u2 // 3, 0, 128, h, sc_d, None, o2, o2k, qchunk=u2)
            normalize(o1, o1k, tmpo[:], "tmpo", 0)
            normalize(o2, o2k, tmpo2[:], "tmpo2", 4)
            P.op("dve", lambda e: e.scalar_tensor_tensor(out=tmpo[:], in0=tmpo2[:], scalar=lam[:, 0:1], in1=tmpo[:],
                                                         op0=ALU.mult, op1=ALU.add), reads=["tmpo", "tmpo2", "lam0"],
                 writes=["tmpo"])
            P.op("pool", lambda e: e.tensor_tensor(out=tmpo2[:], in0=tmpo[:], in1=tmpo[:], op=ALU.mult), reads=["tmpo"],
                 writes=["tmpo2"])
            P.op("dve", lambda e: e.reduce_sum(out=ssq[:, 0:4], in_=tmpo2[:], axis=AX.X), reads=["tmpo2"], writes=["ssq"])
            P.op("act", lambda e: e.activation(out=ssq[:, 0:4], in_=ssq[:, 0:4], func=AF.Ln, scale=1.0 / 64.0,
                                               bias=float(RMS_EPS)), reads=["ssq"], writes=["ssq"])
            P.op("act", lambda e: e.activation(out=ssq[:, 0:4], in_=ssq[:, 0:4], func=AF.Exp, scale=-0.5),
                 reads=["ssq"], writes=["ssq"])
            P.op("dve", lambda e: e.tensor_tensor(out=tmpo[:], in0=tmpo[:],
                                                  in1=ssq[:, 0:4].unsqueeze(2).to_broadcast([128, 4, 64]), op=ALU.mult),
                 reads=["tmpo", "ssq"], writes=["tmpo"])
            P.op("dve", lambda e, h=h, stg=stg: e.tensor_tensor(out=stg[:, :, h * 64:(h + 1) * 64], in0=tmpo[:], in1=dgb[:],
                                                       op=ALU.mult), reads=["tmpo", "dgb"], writes=[stk])
        stage_out(j, 1, stg, stk)

    if stop_after < 3:
        return
    zero_qt()
    load_wm(2560, 772)
    P.op("dve", lambda e: e.memset(Gc[:], 0.0), writes=["Gc"])
    def fox_pre(j):
        qt = QT[j % 2]
        qkey = ("QT", j % 2)
        FBj = FB[:, j % 2]
        fbk = ("FB", j % 2)
        for c in range(max(1, NHP // 2)):
            proj_q_padded(j, c * 128, qt, qkey, c)
            yield
            proj_fm(j, 256 + c * 128, KT[:, c, j * 512:(j + 1) * 512], ("KT", c, j))
            yield

        def gate_extra(t, bank, bk):
            P.op("dve", lambda e: e.tensor_tensor(out=xg[:, t, :], in0=bank[:, 256:260], in1=bfb[:], op=ALU.add),
                 reads=[bk, "bfb"], writes=["xg"])

        FOXMIN = int(os.environ.get("FOXMIN", "0"))
        proj_v(j, 512, 256)
        yield
        if FOXMIN < 1:
            gtb, gtk = pools["O"].next()
            for t in range(4):
                tt = 4 * j + t
                for kc in range(8):
                    P.op("pe", lambda e, kc=kc, tt=tt, t=t, gtb=gtb: e.matmul(gtb[:, t * 4:(t + 1) * 4],
                                                                             lhsT=hT[:, kc, tt * 128:(tt + 1) * 128],
                                                                             rhs=Wm[:, kc, 768:772], start=(kc == 0),
                                                                             stop=(kc == 7), skip_group_check=True),
                         reads=["Wm", ("hT", j)], writes=[gtk])
            P.op("dve", lambda e, gtb=gtb: e.tensor_tensor(out=xg[:], in0=gtb[:, 0:16].rearrange("p (a b) -> p a b", b=4),
                                                           in1=bfb[:].unsqueeze(1).to_broadcast([128, 4, 4]), op=ALU.add),
                 reads=[gtk, "bfb"], writes=["xg"])
        if FOXMIN < 1 and not os.environ.get("FOXNOACT"):
            P.op("act", lambda e: e.activation(out=lgt[:], in_=xg[:], func=AF.Exp, scale=-1.0), reads=["xg"], writes=["lgt"])
            P.op("act", lambda e: e.activation(out=lgt[:], in_=lgt[:], func=AF.Ln, bias=1.0), reads=["lgt"], writes=["lgt"])
        if os.environ.get("FOXNOG"):
            P.op("dve", lambda e: e.memset(G[:], 0.5), writes=["G"])
            P.op("dve", lambda e: e.memset(Gmid[:], 0.25), writes=["Gmid"])
        else:
            l16 = lgt[:].rearrange("p a b -> p (a b)")
            P.op("dve", lambda e: e.tensor_copy(out=lsp[:, :, 0, :], in_=lgt[:]), reads=["lgt"], writes=["lsp0"])
            P.op("dve", lambda e: e.tensor_tensor(out=lr1[:], in0=lgt[:], in1=lsp[:, :, 0, :], op=ALU.subtract),
                 reads=["lgt", "lsp0"], writes=["lr1"])
            P.op("dve", lambda e: e.tensor_copy(out=lsp[:, :, 1, :], in_=lr1[:]), reads=["lr1"], writes=["lsp1"])
            P.op("dve", lambda e: e.tensor_tensor(out=lr2[:], in0=lr1[:], in1=lsp[:, :, 1, :], op=ALU.subtract),
                 reads=["lr1", "lsp1"], writes=["lr2"])
            P.op("dve", lambda e: e.tensor_copy(out=lsp[:, :, 2, :], in_=lr2[:]), reads=["lr2"], writes=["lsp2"])
            LS = ["lsp0", "lsp1", "lsp2"]
            gb, gbk = pools["O"].next()
            for t in range(4):
                P.op("pe", lambda e, t=t: e.matmul(gb[:, t * 12:(t + 1) * 12], lhsT=trib[:],
                                                   rhs=lsp[:, t, :, :].rearrange("p a b -> p (a b)"), start=True,
                                                   stop=(t == 0), skip_group_check=True), reads=LS + ["trib"], writes=[gbk])
                for t2 in range(t):
                    P.op("pe", lambda e, t=t, t2=t2: e.matmul(gb[:, t * 12:(t + 1) * 12], lhsT=onesb[:],
                                                              rhs=lsp[:, t2, :, :].rearrange("p a b -> p (a b)"),
                                                              start=False, stop=(t2 == t - 1), skip_group_check=True),
                         reads=LS + ["onesb"], writes=[gbk])
            P.op("dve", lambda e: e.reduce_sum(out=lr1[:], in_=gb[:, 0:48].rearrange("p (t s h) -> p t h s", s=3, h=4),
                                               axis=AX.X), reads=[gbk], writes=["lr1"])
            P.op("dve", lambda e, j=j: e.tensor_tensor(out=G[:, 4 * j:4 * j + 4, :], in0=lr1[:],
                                                       in1=Gc[:].unsqueeze(1).to_broadcast([128, 4, 4]), op=ALU.add),
                 reads=["lr1", "Gc"], writes=["G"])
            cb, cbk = pools["O"].next()
            for t in range(4):
                P.op("pe", lambda e, t=t: e.matmul(cb[:, 0:12], lhsT=onesb[:], rhs=lsp[:, t, :, :].rearrange("p a b -> p (a b)"),
                                                   start=(t == 0), stop=(t == 3), skip_group_check=True),
                     reads=LS + ["onesb"], writes=[cbk])
            for t in range(3):
                P.op("pe", lambda e, t=t: e.matmul(cb[:, 12:24], lhsT=(onesb[:] if t < 2 else e0b[:]),
                                                   rhs=lsp[:, t, :, :].rearrange("p a b -> p (a b)"), start=False,
                                                   stop=(t == 2), skip_group_check=True), reads=LS + ["onesb", "e0b"],
                     writes=[cbk])
            P.op("dve", lambda e: e.reduce_sum(out=lr2[:, 0:2, :], in_=cb[:, 0:24].rearrange("p (t s h) -> p t h s", s=3, h=4),
                                               axis=AX.X), reads=[cbk], writes=["lr2"])
            P.op("dve", lambda e: e.tensor_tensor(out=Gmid[:], in0=lr2[:, 1, :], in1=Gc[:], op=ALU.add), reads=["lr2", "Gc"],
                 writes=["Gmid"])
            P.op("dve", lambda e: e.tensor_tensor(out=Gc[:], in0=lr2[:, 0, :], in1=Gc[:], op=ALU.add), reads=["lr2", "Gc"],
                 writes=["Gc"])
        nkt = 4 * (j + 1)
        for h in range(NHP if FOXMIN < 2 else 0):
            P.op("dve", lambda e, h=h, nkt=nkt, FBj=FBj: e.tensor_scalar(out=FBj[:, h, 0:nkt], in0=G[:, 0:nkt, h],
                                                                         scalar1=Gmid[:, h:h + 1], scalar2=60.0,
                                                                         op0=ALU.subtract, op1=ALU.min),
                 reads=["G", "Gmid"], writes=[fbk])
        yield

    def fox_att(j):
        qt = QT[j % 2]
        qkey = ("QT", j % 2)
        FBj = FB[:, j % 2]
        fbk = ("FB", j % 2)
        stg, stk = stage.next()
        for h in range(NHP):
            ob, obk = ObA.next()
            softmax_head(j, qt, qkey, h // 2, 0, 128, h, 0.125, (lambda kt, h=h, FBj=FBj: FBj[:, h, kt:kt + 1]), ob, obk,
                         qchunk=h, biaskey=fbk)
            normalize(ob, obk, tmpo[:], "tmpo", 0)
            P.op("act", lambda e, h=h, stg=stg: e.copy(out=stg[:, :, h * 64:(h + 1) * 64], in_=tmpo[:]), reads=["tmpo"],
                 writes=[stk])
        stage_out(j, 2, stg, stk)

    run_pipelined(fox_pre, fox_att)

    if stop_after < 4:
        return
    load_wm(3332, 640)
    sc_m = 1.0 / math.sqrt(96.0)
    def mla_pre(j):
        qt = QT[j % 2]
        qkey = ("QT", j % 2)
        rope_tables(j)
        yield
        for t in range(4):
            tt = 4 * j + t
            bank, bk = pools["O"].next()
            for kc in range(8):
                P.op("pe", lambda e, kc=kc, bank=bank, tt=tt: e.matmul(bank[:, 0:384], lhsT=hT[:, kc, tt * 128:(tt + 1) * 128],
                                                                       rhs=Wm[:, kc, 0:384], start=(kc == 0),
                                                                       stop=(kc == 7)),
                     reads=["Wm", ("hT", j)], writes=[bk])
            P.op("act", lambda e, bank=bank: e.activation(out=junk[:, 0:256], in_=bank[:, 0:256], func=AF.Square,
                                                          accum_out=ssq[:, 4:5]), reads=[bk], writes=["junk", "ssq4"])
            P.op("act", lambda e, bank=bank: e.activation(out=junk[:, 0:128], in_=bank[:, 256:384], func=AF.Square,
                                                          accum_out=ssq[:, 5:6]), reads=[bk], writes=["junk", "ssq5"])
            P.op("act", lambda e: e.activation(out=ssq[:, 4:5], in_=ssq[:, 4:5], func=AF.Ln, scale=1.0 / 256.0,
                                               bias=float(RMS_EPS)), reads=["ssq4"], writes=["ssq4"])
            P.op("act", lambda e: e.activation(out=ssq[:, 5:6], in_=ssq[:, 5:6], func=AF.Ln, scale=1.0 / 128.0,
                                               bias=float(RMS_EPS)), reads=["ssq5"], writes=["ssq5"])
            P.op("act", lambda e: e.activation(out=ssq[:, 4:6], in_=ssq[:, 4:6], func=AF.Exp, scale=-0.5),
                 reads=["ssq4", "ssq5"], writes=["ssq4", "ssq5"])
            P.op("dve", lambda e, bank=bank, t=t: e.scalar_tensor_tensor(out=cqs[:, t, 0:256], in0=bank[:, 0:256],
                                                                         scalar=ssq[:, 4:5], in1=qgb[:], op0=ALU.mult,
                                                                         op1=ALU.mult), reads=[bk, "ssq4", "qgb"],
                 writes=[("cqs", t)])
            P.op("dve", lambda e, bank=bank, t=t: e.scalar_tensor_tensor(out=cqs[:, t, 256:384], in0=bank[:, 256:384],
                                                                         scalar=ssq[:, 5:6], in1=kvgb[:], op0=ALU.mult,
                                                                         op1=ALU.mult), reads=[bk, "ssq5", "kvgb"],
                 writes=[("cqs", t)])
            for c in range(3):
                P.op("pe", lambda e, t=t, c=c: e.transpose(out=Tb[:, c, :], in_=cqs[:, t, c * 128:(c + 1) * 128],
                                                           identity=identb[:]), reads=[("cqs", t), "identb"],
                     writes=["Tb"])
            P.op("dve", lambda e, t=t: e.tensor_copy(out=cqT[:, :, t * 128:(t + 1) * 128], in_=Tb[:, 0:3, :]),
                 reads=["Tb"], writes=["cqT"])
            yield
        for t in range(4):
            tt = 4 * j + t
            bank, bk = pools["O"].next()
            P.op("pe", lambda e, bank=bank, t=t: e.matmul(bank[:, 0:256], lhsT=cqT[:, 2, t * 128:(t + 1) * 128],
                                                          rhs=Wukv[:, 512:768], start=True, stop=True),
                 reads=["cqT", "Wukv"], writes=[bk])
            P.op("act", lambda e, bank=bank, tt=tt: e.copy(out=V[:, tt, :, 0:64],
                                                           in_=bank[:, 0:256].rearrange("p (a b) -> p a b", b=64)),
                 reads=[bk, "Vall"], writes=[("V", j)])
            yield
        bA, kA = proj_fm(j, 384, None, None, post=True)
        bB, kB = proj_fm(j, 512, None, None, post=True)
        rope_evac(bA, kA, bB, kB, krope[64:96, :], "krope", 64, 96)
        for h in range(NHP):
            bank, bk = pools["S"].next()
            P.op("pe", lambda e, bank=bank, h=h: e.matmul(bank[:], lhsT=Wukv[:, h * 128:(h + 1) * 128], rhs=cqT[:, 2, :],
                                                          start=True, stop=True), reads=["cqT", "Wukv"], writes=[bk])
            P.op("act", lambda e, bank=bank, h=h, j=j: e.copy(out=KT[0:64, h, j * 512:(j + 1) * 512], in_=bank[0:64, :]),
                 reads=[bk], writes=[("KT", h, j)])
            P.op("pool", lambda e, h=h, j=j: e.tensor_copy(out=KT[64:96, h, j * 512:(j + 1) * 512], in_=krope[64:96, :]),
                 reads=["krope", ("KT", h, j)], writes=[("KT", h, j)])
            bA, kA = pools["S"].next()
            bB, kB = pools["S"].next()
            for c2 in range(2):
                P.op("pe", lambda e, bA=bA, c2=c2, h=h: e.matmul(bA[:], lhsT=Wuq[:, c2, h * 256:h * 256 + 128],
                                                                 rhs=cqT[:, c2, :], start=(c2 == 0), stop=(c2 == 1)),
                     reads=["cqT", "Wuq"], writes=[kA])
            for c2 in range(2):
                P.op("pe", lambda e, bB=bB, c2=c2, h=h: e.matmul(bB[:], lhsT=Wuq[:, c2, h * 256 + 128:h * 256 + 256],
                                                                 rhs=cqT[:, c2, :], start=(c2 == 0), stop=(c2 == 1)),
                     reads=["cqT", "Wuq"], writes=[kB])
            P.op("act", lambda e, bA=bA, h=h, qt=qt: e.copy(out=qt[0:64, h, :], in_=bA[0:64, :]), reads=[kA], writes=[qkey])
            rope_evac(bA, kA, bB, kB, qt[64:96, h, :], qkey, 64, 96)
            yield

    def mla_att(j):
        qt = QT[j % 2]
        qkey = ("QT", j % 2)
        stg, stk = stage.next()
        for h in range(NHP):
            ob, obk = ObA.next()
            softmax_head(j, qt, qkey, h, 0, 96, h, sc_m, None, ob, obk)
            normalize(ob, obk, tmpo[:], "tmpo", 0)
            P.op("act", lambda e, h=h, stg=stg: e.copy(out=stg[:, :, h * 64:(h + 1) * 64], in_=tmpo[:]), reads=["tmpo"],
                 writes=[stk])
        stage_out(j, 3, stg, stk)

    run_pipelined(mla_pre, mla_att)


def declare_a_inputs(nc, S, pre_ln, sfx=""):
    T = {}

    def din(name, shape, dt=F32):
        T[name] = nc.dram_tensor(name + sfx, list(shape), dt, kind="ExternalInput").ap()

    din("hin", [S, D])
    din("pos", [S], I32)
    din("win", [D, NWIN])
    din("wuq", [256, 1024])
    din("wukv", [128, 768])
    din("bf", [4])
    din("dlam", [128])
    din("dg", [64])
    din("qg", [256])
    din("kvg", [128])
    if pre_ln:
        din("lng", [D])
        din("lnb", [D])
    return T


def declare_consts(nc, T):
    for name, shape in (("ident", [128, 128]), ("maskd", [128, 128]), ("masks", [128, 128]), ("negtri", [128, 128]),
                        ("triincl", [128, 128]), ("ones", [128, 128]), ("e0", [128, 128]), ("ropef", [128, 2]),
                        ("iota_e", [128, NE]), ("tristrict", [128, 128])):
        T[name] = nc.dram_tensor(name, shape, F32, kind="ExternalInput").ap()


def build_a(S, pre_ln, lam_init, stop_after=9, NHP=4):
    nc = bass.Bass("TRN2", target_bir_lowering=False)
    T = declare_a_inputs(nc, S, pre_ln)
    declare_consts(nc, T)
    T["mixed"] = nc.dram_tensor("mixed", [S, D], BF16, kind="ExternalOutput").ap()
    if pre_ln:
        T["h0"] = nc.dram_tensor("h0", [S, D], F32, kind="ExternalOutput").ap()
    with ExitStack() as es:
        P = Prog(nc, es)
        phase_a(nc, P, es, S, T, pre_ln, lam_init, "a_", stop_after, NHP)
        P.finish("sp")
        P.emit()
    return nc


def a_inputs(l, inputs, hin, pos, pre_ln, hp=(0, 1, 2, 3)):
    m = dict(hin=np.ascontiguousarray(hin, dtype=np.float32), pos=np.ascontiguousarray(pos, dtype=np.int32),
             win=prep_w_in(np.asarray(inputs["w_in"][l]), hp), wuq=prep_w_uq(np.asarray(inputs["mla_w_uq"][l]), hp),
             wukv=prep_w_ukv(np.asarray(inputs["mla_w_ukv"][l]), hp),
             bf=np.ascontiguousarray(np.asarray(inputs["b_forget"][l])[list(hp)], dtype=np.float32),
             dlam=np.ascontiguousarray(np.asarray(inputs["diff_lambda"][l]).reshape(128), dtype=np.float32),
             dg=np.ascontiguousarray(inputs["diff_subln_g"][l], dtype=np.float32),
             qg=np.ascontiguousarray(inputs["mla_q_norm_g"][l], dtype=np.float32),
             kvg=np.ascontiguousarray(inputs["mla_kv_norm_g"][l], dtype=np.float32))
    if pre_ln:
        m["lng"] = np.ascontiguousarray(inputs["ln_in_g"], dtype=np.float32)
        m["lnb"] = np.ascontiguousarray(inputs["ln_in_b"], dtype=np.float32)
    m.update(host_consts())
    return m


def phase_b(nc, P, es, NTOK, C, T, tag, stop_after=9):
    NTT = NTOK // 128
    CT = C // 128
    NSLOT = NE * C
    RNG = [(n0, min(512, C - n0)) for n0 in range(0, C, 512)]

    es1, es2, es3 = ExitStack(), ExitStack(), ExitStack()
    cur = [es]

    def sb(name, shape, dt):
        return cur[0].enter_context(nc.sbuf_tensor(tag + name, shape, dt))

    def ps(name, shape, dt):
        return es.enter_context(nc.psum_tensor(tag + name, shape, dt))

    K_ = lambda s_: tag + s_
    identb = sb("identb", [128, 128], BF16)
    onesb = sb("onesb", [128, 128], BF16)
    tsb = sb("tsb", [128, 128], BF16)
    ecap = sb("ecap", [128, NE], F32)
    bguT = sb("bguT", [128, 16, NE], F32)
    bguT2 = sb("bguT2", [128, 16, NE], F32)
    cnt = sb("cnt", [128, NE], F32)
    slotk = sb("slotk", [128, NTT, 4], F32)
    gk = sb("gk", [128, NTT, 4], F32)
    idx = sb("idx", [128, NTT, 4], I32)
    zer = sb("zer", [128, 1024], F32)
    st6 = sb("st6", [128, 2, 6], F32)
    mv = sb("mv", [128, 2], F32)
    ridx = sb("ridx", [128, NTT], I32)
    ridg0 = sb("ridg0", [128, NTT], I32)
    ridg1 = sb("ridg1", [128, NTT], I32)
    cur[0] = es1
    Wout = sb("Wout", [128, 8, 1024], BF16)
    g1b = sb("g1b", [128, 1024], F32)
    b1b = sb("b1b", [128, 1024], F32)
    rbb = sb("rbb", [128, NE], F32)
    rwf = sb("rwf", [128, 8, NE], F32)
    rwr = sb("rwr", [128, 8, NE], F32)
    rw0 = sb("rw0", [128, 8, NE], BF16)
    rw1 = sb("rw1", [128, 8, NE], BF16)
    bguf = sb("bguf", [NE, 2048], F32)
    bgur = sb("bgur", [NE, 2048], F32)
    bgu0 = sb("bgu0", [NE, 2048], BF16)
    bgu1 = sb("bgu1", [NE, 2048], BF16)
    mtl = Rot([(sb(f"mt{i}", [128, 1024], BF16), K_(f"mt{i}")) for i in range(4)])
    mTs = Rot([(sb(f"mT{i}", [128, 8, 128], BF16), K_(f"mT{i}")) for i in range(2)])
    hts = Rot([(sb(f"ht{i}", [128, 1024], F32), K_(f"ht{i}")) for i in range(2)])
    rts = Rot([(sb(f"rt{i}", [128, 1024], F32), K_(f"rt{i}")) for i in range(2)])
    h1bs = Rot([(sb(f"h1b{i}", [128, 1024], BF16), K_(f"h1b{i}")) for i in range(2)])
    h1m = sb("h1m", [128, 1024], BF16)
    hres_ = sb("hres_", [128, 1024], F32)
    a0T = sb("a0T", [128, 8, 128], BF16)
    a1T = sb("a1T", [128, 8, 128], BF16)
    lg = sb("lg", [128, NE], F32)
    mx8 = sb("mx8", [128, 8], F32)
    msk = sb("msk", [128, NE], F32)
    mskb = sb("mskb", [128, NE], BF16)
    nm = sb("nm", [128, 1], F32)
    ex = sb("ex", [128, NE], F32)
    ssum = sb("ssum", [128, 1], F32)
    gate = sb("gate", [128, NE], F32)
    pos = sb("pos", [128, NE], F32)
    okm = sb("okm", [128, NE], F32)
    slot = sb("slot", [128, NE], F32)
    sel = sb("sel", [128, NE], F32)
    tmp32 = sb("tmp32", [128, NE], F32)
    Sb = Rot([(ps(f"S{i}", [128, 512], F32), K_(f"S{i}")) for i in range(3)])
    Ob = Rot([(ps(f"O{i}", [128, 512], F32), K_(f"O{i}")) for i in range(4)])
    Tb = ps("Tb", [128, 8, 128], BF16)
    TbK = K_("Tb")

    def ld(q, dst, src, key):
        P.dma(q, lambda e: e.dma_start(out=dst, in_=src), writes=[key])

    ld("pool", identb[:], T["ident"], K_("identb"))
    ld("pool", onesb[:], T["ones"], K_("onesb"))
    ld("pool", tsb[:], T["tristrict"], K_("tsb"))
    ld("sp", ecap[:], T["iota_e"], K_("ecap"))
    ld("sp", g1b[:], T["ln1g"].partition_broadcast(128), K_("g1b"))
    ld("sp", b1b[:], T["ln1b"].partition_broadcast(128), K_("b1b"))
    ld("sp", rbb[:], T["rb"].partition_broadcast(128), K_("rbb"))
    ld("sp", rwf[:], T["rw"].rearrange("(c p) n -> p c n", p=128), K_("rwf"))
    ld("sp", bguf[:], T["bgu"], K_("bguf"))
    ld("pool", Wout[:], T["wout"].rearrange("(c p) n -> p c n", p=128), K_("Wout"))
    P.op("dve", lambda e: e.tensor_scalar(out=ecap[:], in0=ecap[:], scalar1=float(C), scalar2=None, op0=ALU.mult),
         reads=[K_("ecap")], writes=[K_("ecap")])
    P.op("dve", lambda e: e.memset(cnt[:], 0.0), writes=[K_("cnt")])
    P.op("dve", lambda e: e.memset(zer[:], 0.0), writes=[K_("zer")])
    zb16 = zer[:].bitcast(BF16)
    P.dma("sp", lambda e: e.dma_start(out=T["ys"][NSLOT:NSLOT + 128, :], in_=zb16[:, 0:1024]), reads=[K_("zer")],
          writes=[K_("ysd")])
    for e_ in range(NE):
        for a_ in range(0, CT, 2):
            na = min(2, CT - a_)
            r0 = e_ * C + a_ * 128
            P.dma("sp", lambda e, r0=r0, na=na: e.dma_start(
                out=T["xs"][r0:r0 + na * 128, :].rearrange("(a p) f -> p a f", p=128),
                in_=zb16[:, 0:na * 1024].rearrange("p (a f) -> p a f", f=1024)), reads=[K_("zer")], writes=[K_(f"xsz{e_}_{a_}")])
    XSZ = [K_(f"xsz{e_}_{a_}") for e_ in range(NE) for a_ in range(0, CT, 2)]
    P.op("dve", lambda e: e.tensor_copy(out=rw0[:], in_=rwf[:]), reads=[K_("rwf")], writes=[K_("rw0")])
    P.op("dve", lambda e: e.tensor_tensor(out=rwr[:], in0=rwf[:], in1=rw0[:], op=ALU.subtract),
         reads=[K_("rwf"), K_("rw0")], writes=[K_("rwr")])
    P.op("dve", lambda e: e.tensor_copy(out=rw1[:], in_=rwr[:]), reads=[K_("rwr")], writes=[K_("rw1")])
    P.op("dve", lambda e: e.tensor_copy(out=bgu0[:], in_=bguf[:]), reads=[K_("bguf")], writes=[K_("bgu0")])
    P.op("dve", lambda e: e.tensor_tensor(out=bgur[:], in0=bguf[:], in1=bgu0[:], op=ALU.subtract),
         reads=[K_("bguf"), K_("bgu0")], writes=[K_("bgur")])
    P.op("dve", lambda e: e.tensor_copy(out=bgu1[:], in_=bgur[:]), reads=[K_("bgur")], writes=[K_("bgu1")])
    for part, (src, srck, dst) in enumerate(((bgu0, "bgu0", bguT), (bgu1, "bgu1", bguT2))):
        for g_ in range(2):
            for c in range(8):
                cc = g_ * 8 + c
                P.op("pe", lambda e, src=src, c=c, cc=cc: e.transpose(out=Tb[:, c, 0:NE], in_=src[:, cc * 128:(cc + 1) * 128],
                                                                      identity=identb[0:NE, 0:NE]),
                     reads=[K_(srck), K_("identb")], writes=[TbK])
            P.op("dve", lambda e, dst=dst, g_=g_: e.tensor_copy(out=dst[:, g_ * 8:(g_ + 1) * 8, :], in_=Tb[:, :, 0:NE]),
                 reads=[TbK], writes=[K_("bguT%d" % part)])
    P.op("dve", lambda e: e.tensor_tensor(out=bguT[:], in0=bguT[:], in1=bguT2[:], op=ALU.add),
         reads=[K_("bguT0"), K_("bguT1")], writes=[K_("bguT0")])

    def layer_norm_tile(x, xk, gb, bb, gk_, bk_):
        for c in range(2):
            P.op("dve", lambda e, c=c: e.bn_stats(out=st6[:, c, :], in_=x[:, c * 512:(c + 1) * 512]), reads=[xk],
                 writes=[K_("st6")])
        P.op("dve", lambda e: e.bn_aggr(out=mv[:], in_=st6[:].rearrange("p a b -> p (a b)")), reads=[K_("st6")],
             writes=[K_("mv")])
        P.op("act", lambda e: e.activation(out=mv[:, 1:2], in_=mv[:, 1:2], func=AF.Ln, bias=float(LN_EPS)),
             reads=[K_("mv")], writes=[K_("mv")])
        P.op("act", lambda e: e.activation(out=mv[:, 1:2], in_=mv[:, 1:2], func=AF.Exp, scale=-0.5), reads=[K_("mv")],
             writes=[K_("mv")])
        P.op("dve", lambda e: e.scalar_tensor_tensor(out=x[:], in0=x[:], scalar=mv[:, 0:1], in1=gb[:], op0=ALU.subtract,
                                                     op1=ALU.mult), reads=[xk, K_("mv"), gk_], writes=[xk])
        P.op("dve", lambda e: e.scalar_tensor_tensor(out=x[:], in0=x[:], scalar=mv[:, 1:2], in1=bb[:], op0=ALU.mult,
                                                     op1=ALU.add), reads=[xk, K_("mv"), bk_], writes=[xk])

    use_idx = "rowidx" in T
    if use_idx:
        ld("sp", ridx[:], T["rowidx"], K_("ridx"))
    use_g = "mixG" in T
    if use_g:
        ld("sp", ridg0[:], T["rowidxg0"], K_("ridg0"))
        ld("sp", ridg1[:], T["rowidxg1"], K_("ridg1"))
    for t in range(NTT):
        mt, mk = mtl.next()
        mT, mTk = mTs.next()
        ht, hk = hts.next()
        rt, rk = rts.next()
        h1b, h1bk = h1bs.next()
        if use_g:
            mt1, mk1 = mtl.next()
            P.dma("pool", lambda e, mt=mt, t=t: e.indirect_dma_start(
                out=mt[:], out_offset=None, in_=T["mixG"],
                in_offset=bass.IndirectOffsetOnAxis(ap=ridg0[:, t:t + 1], axis=0)), reads=[K_("ridg0")], writes=[mk])
            P.dma("pool", lambda e, mt1=mt1, t=t: e.indirect_dma_start(
                out=mt1[:], out_offset=None, in_=T["mixG"],
                in_offset=bass.IndirectOffsetOnAxis(ap=ridg1[:, t:t + 1], axis=0)), reads=[K_("ridg1")], writes=[mk1])
        elif use_idx:
            P.dma("pool", lambda e, mt=mt, t=t: e.indirect_dma_start(
                out=mt[:], out_offset=None, in_=T["mixed"],
                in_offset=bass.IndirectOffsetOnAxis(ap=ridx[:, t:t + 1], axis=0)), reads=[K_("ridx")], writes=[mk])
        else:
            P.dma("sp", lambda e, mt=mt, t=t: e.dma_start(out=mt[:], in_=T["mixed"][t * 128:(t + 1) * 128, :]), writes=[mk])
        if "hres_local" in T:
            P.dma("sp", lambda e, ht=ht, t=t: e.dma_start(out=ht[:], in_=T["hres_local"][t * 128:(t + 1) * 128, :]),
                  writes=[hk])
        elif use_idx:
            P.dma("pool", lambda e, ht=ht, t=t: e.indirect_dma_start(
                out=ht[:], out_offset=None, in_=T["hres"],
                in_offset=bass.IndirectOffsetOnAxis(ap=ridx[:, t:t + 1], axis=0)), reads=[K_("ridx")], writes=[hk])
        else:
            P.dma("sp", lambda e, ht=ht, t=t: e.dma_start(out=ht[:], in_=T["hres"][t * 128:(t + 1) * 128, :]), writes=[hk])
        for c in range(8):
            if use_g:
                srct, srck = (mt, mk) if c % 2 == 0 else (mt1, mk1)
                c0 = (c // 2) * 256
            else:
                srct, srck, c0 = mt, mk, c * 128
            P.op("pe", lambda e, srct=srct, c=c, c0=c0: e.transpose(out=Tb[:, c, :], in_=srct[:, c0:c0 + 128],
                                                                    identity=identb[:]),
                 reads=[srck, K_("identb")], writes=[TbK])
        P.op("act", lambda e, mT=mT: e.copy(out=mT[:], in_=Tb[:]), reads=[TbK], writes=[mTk])
        for half in range(2):
            bank, bk = Sb.next()
            for c in range(8):
                P.op("pe", lambda e, bank=bank, c=c, half=half, mT=mT: e.matmul(
                    bank[:], lhsT=mT[:, c, :], rhs=Wout[:, c, half * 512:(half + 1) * 512], start=(c == 0), stop=(c == 7)),
                    reads=[mTk, K_("Wout")], writes=[bk])
            P.op("dve", lambda e, bank=bank, half=half, ht=ht, rt=rt: e.scalar_tensor_tensor(
                out=rt[:, half * 512:(half + 1) * 512], in0=ht[:, half * 512:(half + 1) * 512], scalar=float(ALPHA),
                in1=bank[:], op0=ALU.mult, op1=ALU.add), reads=[bk, hk], writes=[rk])
        layer_norm_tile(rt, rk, g1b, b1b, K_("g1b"), K_("b1b"))
        P.dma("act", lambda e, rt=rt, t=t: e.dma_start(out=T["h1s"][t * 128:(t + 1) * 128, :], in_=rt[:]), reads=[rk])
        P.op("act", lambda e, rt=rt, h1b=h1b: e.copy(out=h1b[:], in_=rt[:]), reads=[rk], writes=[h1bk])
        P.op("dve", lambda e, rt=rt, h1b=h1b: e.tensor_tensor(out=hres_[:], in0=rt[:], in1=h1b[:], op=ALU.subtract),
             reads=[rk, h1bk], writes=[K_("hres_")])
        P.op("act", lambda e: e.copy(out=h1m[:], in_=hres_[:]), reads=[K_("hres_")], writes=[K_("h1m")])
        for src, srck, dst, dstk in ((h1b, h1bk, a0T, K_("a0T")), (h1m, K_("h1m"), a1T, K_("a1T"))):
            for c in range(8):
                P.op("pe", lambda e, src=src, c=c: e.transpose(out=Tb[:, c, :], in_=src[:, c * 128:(c + 1) * 128],
                                                               identity=identb[:]), reads=[srck, K_("identb")], writes=[TbK])
            P.op("dve", lambda e, dst=dst: e.tensor_copy(out=dst[:], in_=Tb[:]), reads=[TbK], writes=[dstk])
        lb, lbk = Ob.next()
        n_mm = 0
        for (aT, aTk, rwx, rwk) in ((a0T, K_("a0T"), rw0, K_("rw0")), (a0T, K_("a0T"), rw1, K_("rw1")),
                                    (a1T, K_("a1T"), rw0, K_("rw0"))):
            for c in range(8):
                P.op("pe", lambda e, aT=aT, rwx=rwx, c=c, lb=lb, n_mm=n_mm: e.matmul(
                    lb[:, 0:NE], lhsT=aT[:, c, :], rhs=rwx[:, c, :], start=(n_mm == 0), stop=(n_mm == 23)),
                    reads=[aTk, rwk], writes=[lbk])
                n_mm += 1
        P.op("dve", lambda e, lb=lb: e.tensor_tensor(out=lg[:], in0=lb[:, 0:NE], in1=rbb[:], op=ALU.add),
             reads=[lbk, K_("rbb")], writes=[K_("lg")])
        P.op("dve", lambda e: e.max(out=mx8[:], in_=lg[:]), reads=[K_("lg")], writes=[K_("mx8")])
        P.op("dve", lambda e: e.tensor_scalar(out=msk[:], in0=lg[:], scalar1=mx8[:, 3:4], scalar2=None, op0=ALU.is_ge),
             reads=[K_("lg"), K_("mx8")], writes=[K_("msk")])
        P.op("dve", lambda e: e.tensor_scalar(out=nm[:], in0=mx8[:, 0:1], scalar1=-1.0, scalar2=None, op0=ALU.mult),
             reads=[K_("mx8")], writes=[K_("nm")])
        P.op("act", lambda e: e.activation(out=ex[:], in_=lg[:], func=AF.Exp, bias=nm[:, 0:1], scale=1.0),
             reads=[K_("lg"), K_("nm")], writes=[K_("ex")])
        P.op("dve", lambda e: e.tensor_tensor(out=ex[:], in0=ex[:], in1=msk[:], op=ALU.mult), reads=[K_("ex"), K_("msk")],
             writes=[K_("ex")])
        P.op("dve", lambda e: e.reduce_sum(out=ssum[:], in_=ex[:], axis=AX.X), reads=[K_("ex")], writes=[K_("ssum")])
        P.op("dve", lambda e: e.reciprocal(out=ssum[:], in_=ssum[:]), reads=[K_("ssum")], writes=[K_("ssum")])
        P.op("dve", lambda e: e.tensor_scalar(out=gate[:], in0=ex[:], scalar1=ssum[:, 0:1], scalar2=None, op0=ALU.mult),
             reads=[K_("ex"), K_("ssum")], writes=[K_("gate")])
        P.op("act", lambda e: e.copy(out=mskb[:], in_=msk[:]), reads=[K_("msk")], writes=[K_("mskb")])
        cb, cbk = Ob.next()
        P.op("pe", lambda e, cb=cb: e.matmul(cb[:, 0:NE], lhsT=tsb[:], rhs=mskb[:], start=True, stop=True),
             reads=[K_("tsb"), K_("mskb")], writes=[cbk])
        P.op("pe", lambda e, cb=cb: e.matmul(cb[:, NE:2 * NE], lhsT=onesb[:], rhs=mskb[:], start=False, stop=True,
                                             skip_group_check=True), reads=[K_("onesb"), K_("mskb")], writes=[cbk])
        P.op("dve", lambda e, cb=cb: e.tensor_tensor(out=pos[:], in0=cb[:, 0:NE], in1=cnt[:], op=ALU.add),
             reads=[cbk, K_("cnt")], writes=[K_("pos")])
        P.op("dve", lambda e, cb=cb: e.tensor_tensor(out=cnt[:], in0=cb[:, NE:2 * NE], in1=cnt[:], op=ALU.add),
             reads=[cbk, K_("cnt")], writes=[K_("cnt")])
        P.op("dve", lambda e: e.tensor_scalar(out=okm[:], in0=pos[:], scalar1=float(C), scalar2=None, op0=ALU.is_lt),
             reads=[K_("pos")], writes=[K_("okm")])
        P.op("dve", lambda e: e.tensor_tensor(out=slot[:], in0=pos[:], in1=ecap[:], op=ALU.add),
             reads=[K_("pos"), K_("ecap")], writes=[K_("slot")])
        P.op("dve", lambda e: e.tensor_scalar(out=slot[:], in0=slot[:], scalar1=-float(NSLOT), scalar2=None, op0=ALU.add),
             reads=[K_("slot")], writes=[K_("slot")])
        P.op("dve", lambda e: e.tensor_tensor(out=slot[:], in0=slot[:], in1=okm[:], op=ALU.mult),
             reads=[K_("slot"), K_("okm")], writes=[K_("slot")])
        P.op("dve", lambda e: e.tensor_scalar(out=slot[:], in0=slot[:], scalar1=float(NSLOT), scalar2=None, op0=ALU.add),
             reads=[K_("slot")], writes=[K_("slot")])
        for k in range(4):
            P.op("dve", lambda e, k=k: e.tensor_scalar(out=sel[:], in0=lg[:], scalar1=mx8[:, k:k + 1], scalar2=None,
                                                       op0=ALU.is_equal), reads=[K_("lg"), K_("mx8")], writes=[K_("sel")])
            P.op("dve", lambda e: e.tensor_tensor(out=tmp32[:], in0=sel[:], in1=slot[:], op=ALU.mult),
                 reads=[K_("sel"), K_("slot")], writes=[K_("tmp32")])
            P.op("dve", lambda e, k=k, t=t: e.reduce_sum(out=slotk[:, t, k:k + 1], in_=tmp32[:], axis=AX.X),
                 reads=[K_("tmp32")], writes=[K_("slotk")])
            P.op("dve", lambda e: e.tensor_tensor(out=tmp32[:], in0=sel[:], in1=gate[:], op=ALU.mult),
                 reads=[K_("sel"), K_("gate")], writes=[K_("tmp32")])
            P.op("dve", lambda e, k=k, t=t: e.reduce_sum(out=gk[:, t, k:k + 1], in_=tmp32[:], axis=AX.X),
                 reads=[K_("tmp32")], writes=[K_("gk")])
        P.op("dve", lambda e, t=t: e.tensor_copy(out=idx[:, t, :], in_=slotk[:, t, :]), reads=[K_("slotk")],
             writes=[K_("idx")])
        for k in range(4):
            P.dma("pool", lambda e, k=k, t=t, h1b=h1b: e.indirect_dma_start(
                out=T["xs"], out_offset=bass.IndirectOffsetOnAxis(ap=idx[:, t, k:k + 1], axis=0), in_=h1b[:],
                in_offset=None), reads=[h1bk, K_("idx")] + XSZ)
    P.barrier()
    es1.close()
    if stop_after < 1:
        return
    cur[0] = es2
    Wgu = [sb(f"Wgu{i}", [128, 8, 2048], BF16) for i in range(2)]
    Wd = [sb(f"Wd{i}", [128, 8, 1024], BF16) for i in range(2)]
    bdb = [sb(f"bdb{i}", [128, 1024], F32) for i in range(2)]
    xsl = [sb(f"xsl{i}", [128, CT, 1024], BF16) for i in range(2)]
    xT = sb("xT", [128, 8, C], BF16)
    gT = sb("gT", [128, 8, C], BF16)
    W1 = Rot([(sb(f"w1_{i}", [128, 512], F32), K_(f"w1_{i}")) for i in range(2)])
    W2 = Rot([(sb(f"w2_{i}", [128, 512], F32), K_(f"w2_{i}")) for i in range(2)])
    W3 = Rot([(sb(f"w3_{i}", [128, 512], F32), K_(f"w3_{i}")) for i in range(2)])
    ysb = Rot([(sb(f"ysb{i}", [128, 1024], BF16), K_(f"ysb{i}")) for i in range(2)])
    stg32 = Rot([(sb(f"stg32_{i}", [128, 2048], F32), K_(f"stg32_{i}")) for i in range(4)])

    def load_expert(e_):
        p_ = e_ % 2
        P.dma("sp", lambda e, p_=p_, e_=e_: e.dma_start(out=bdb[p_][:], in_=T["bd"][e_].partition_broadcast(128)),
              writes=[K_(f"bdb{p_}")])
        P.dma("sp", lambda e, p_=p_, e_=e_: e.dma_start(
            out=xsl[p_][:], in_=T["xs"][e_ * C:(e_ + 1) * C, :].rearrange("(a p) f -> p a f", p=128)),
            writes=[K_(f"xsl{p_}")])
        ops = []
        for c in range(8):
            def f(c=c):
                st_, sk = stg32.next()
                P.dma("sp", lambda e, st_=st_, c=c: e.dma_start(out=st_[:], in_=T["wgu"][e_, c * 128:(c + 1) * 128, :]),
                      writes=[sk])
                if c % 2:
                    P.op("act", lambda e, st_=st_, c=c: e.copy(out=Wgu[p_][:, c, :], in_=st_[:]), reads=[sk],
                         writes=[K_(f"Wgu{p_}_{c}")])
                else:
                    P.op("dve", lambda e, st_=st_, c=c: e.tensor_copy(out=Wgu[p_][:, c, :], in_=st_[:]), reads=[sk],
                         writes=[K_(f"Wgu{p_}_{c}")])
            ops.append(f)
        for c2 in range(4):
            def f(c2=c2):
                st_, sk = stg32.next()
                P.dma("sp", lambda e, st_=st_, c2=c2: e.dma_start(
                    out=st_[:].rearrange("p (a n) -> p a n", a=2),
                    in_=T["wd"][e_, c2 * 256:(c2 + 1) * 256, :].rearrange("(a p) n -> p a n", p=128)), writes=[sk])
                if c2 % 2:
                    P.op("act", lambda e, st_=st_, c2=c2: e.copy(out=Wd[p_][:, 2 * c2:2 * c2 + 2, :],
                                                                 in_=st_[:].rearrange("p (a n) -> p a n", a=2)),
                         reads=[sk], writes=[K_(f"Wd{p_}_{c2}")])
                else:
                    P.op("dve", lambda e, st_=st_, c2=c2: e.tensor_copy(out=Wd[p_][:, 2 * c2:2 * c2 + 2, :],
                                                                        in_=st_[:].rearrange("p (a n) -> p a n", a=2)),
                         reads=[sk], writes=[K_(f"Wd{p_}_{c2}")])
            ops.append(f)
        return ops

    pend_ops = load_expert(0)
    for f in pend_ops:
        f()
    pend_ops = []

    def pump(n=1):
        for _ in range(n):
            if pend_ops:
                pend_ops.pop(0)()

    for e_ in range(NE):
        p_ = e_ % 2
        if e_ + 1 < NE:
            pend_ops.extend(load_expert(e_ + 1))
        for a in range(CT):
            for c in range(8):
                P.op("pe", lambda e, p_=p_, a=a, c=c: e.transpose(out=Tb[:, c, :], in_=xsl[p_][:, a, c * 128:(c + 1) * 128],
                                                                  identity=identb[:]),
                     reads=[K_(f"xsl{p_}"), K_("identb")], writes=[TbK])
            P.op("act", lambda e, a=a: e.copy(out=xT[:, :, a * 128:(a + 1) * 128], in_=Tb[:]), reads=[TbK],
                 writes=[K_("xT")])
        for (n0, nn) in RNG:
            for fc in range(8):
                gb_, gbk = Ob.next()
                lb_, lbk = Ob.next()
                for (bank, bk_, col) in ((gb_, gbk, fc * 128), (lb_, lbk, 1024 + fc * 128)):
                    for c in range(8):
                        P.op("pe", lambda e, bank=bank, c=c, col=col, p_=p_, n0=n0, nn=nn: e.matmul(
                            bank[:, 0:nn], lhsT=Wgu[p_][:, c, col:col + 128], rhs=xT[:, c, n0:n0 + nn], start=(c == 0),
                            stop=(c == 7)), reads=[K_(f"Wgu{p_}_{c}"), K_("xT")], writes=[bk_])
                w1, w1k = W1.next()
                w2, w2k = W2.next()
                w3, w3k = W3.next()
                P.op("dve", lambda e, gb_=gb_, w1=w1, fc=fc, e_=e_, nn=nn: e.tensor_scalar(
                    out=w1[:, 0:nn], in0=gb_[:, 0:nn], scalar1=bguT[:, fc, e_:e_ + 1], scalar2=7.0, op0=ALU.add, op1=ALU.min),
                    reads=[gbk, K_("bguT0")], writes=[w1k])
                P.op("act", lambda e, w1=w1, w2=w2, nn=nn: e.activation(out=w2[:, 0:nn], in_=w1[:, 0:nn], func=AF.Sigmoid,
                                                                         scale=1.702), reads=[w1k], writes=[w2k])
                P.op("dve", lambda e, lb_=lb_, w3=w3, fc=fc, e_=e_, nn=nn: e.tensor_scalar(
                    out=w3[:, 0:nn], in0=lb_[:, 0:nn], scalar1=bguT[:, 8 + fc, e_:e_ + 1], scalar2=7.0, op0=ALU.add,
                    op1=ALU.min), reads=[lbk, K_("bguT0")], writes=[w3k])
                P.op("dve", lambda e, w3=w3, nn=nn: e.tensor_scalar(out=w3[:, 0:nn], in0=w3[:, 0:nn], scalar1=-7.0,
                                                                    scalar2=1.0, op0=ALU.max, op1=ALU.add),
                     reads=[w3k], writes=[w3k])
                P.op("dve", lambda e, w1=w1, w2=w2, nn=nn: e.tensor_tensor(out=w1[:, 0:nn], in0=w1[:, 0:nn], in1=w2[:, 0:nn],
                                                                           op=ALU.mult), reads=[w1k, w2k], writes=[w1k])
                P.op("dve", lambda e, w1=w1, w3=w3, fc=fc, n0=n0, nn=nn: e.tensor_tensor(
                    out=gT[:, fc, n0:n0 + nn], in0=w1[:, 0:nn], in1=w3[:, 0:nn], op=ALU.mult), reads=[w1k, w3k],
                    writes=[K_("gT")])
                pump(1)
        for a in range(CT):
            yt, ytk = ysb.next()
            for half in range(2):
                bank, bk_ = Sb.next()
                for fc in range(8):
                    P.op("pe", lambda e, bank=bank, fc=fc, a=a, half=half, p_=p_: e.matmul(
                        bank[:], lhsT=gT[:, fc, a * 128:(a + 1) * 128], rhs=Wd[p_][:, fc, half * 512:(half + 1) * 512],
                        start=(fc == 0), stop=(fc == 7)), reads=[K_("gT"), K_(f"Wd{p_}_{fc // 2}")], writes=[bk_])
                P.op("dve", lambda e, bank=bank, yt=yt, half=half, p_=p_: e.tensor_tensor(
                    out=yt[:, half * 512:(half + 1) * 512], in0=bank[:], in1=bdb[p_][:, half * 512:(half + 1) * 512],
                    op=ALU.add), reads=[bk_, K_(f"bdb{p_}")], writes=[ytk])
            r0 = e_ * C + a * 128
            P.dma("sp", lambda e, yt=yt, r0=r0: e.dma_start(out=T["ys"][r0:r0 + 128, :], in_=yt[:]), reads=[ytk])
            pump(1)
        pump(99)
    P.barrier()
    es2.close()
    if stop_after < 2:
        return
    cur[0] = es3
    g2b = sb("g2b", [128, 1024], F32)
    b2b = sb("b2b", [128, 1024], F32)
    yks = [Rot([(sb(f"yk{k}_{i}", [128, 1024], BF16), K_(f"yk{k}_{i}")) for i in range(3)]) for k in range(4)]
    acc = Rot([(sb(f"acc{i}", [128, 1024], F32), K_(f"acc{i}")) for i in range(2)])
    hts = Rot([(sb(f"ht3_{i}", [128, 1024], F32), K_(f"ht3_{i}")) for i in range(2)])
    ld("sp", g2b[:], T["ln2g"].partition_broadcast(128), K_("g2b"))
    ld("sp", b2b[:], T["ln2b"].partition_broadcast(128), K_("b2b"))

    for t in range(NTT):
        ht, hk = hts.next()
        ac, ack = acc.next()
        P.dma("sp", lambda e, ht=ht, t=t: e.dma_start(out=ht[:], in_=T["h1s"][t * 128:(t + 1) * 128, :]),
              writes=[hk])
        for k in range(4):
            yk, ykk = yks[k].next()
            P.dma("pool", lambda e, yk=yk, k=k, t=t: e.indirect_dma_start(
                out=yk[:], out_offset=None, in_=T["ys"],
                in_offset=bass.IndirectOffsetOnAxis(ap=idx[:, t, k:k + 1], axis=0)),
                reads=[K_("idx")], writes=[ykk])
            if k == 0:
                P.op("dve", lambda e, yk=yk, ac=ac, t=t: e.tensor_scalar(out=ac[:], in0=yk[:], scalar1=gk[:, t, 0:1],
                                                                         scalar2=None, op0=ALU.mult),
                     reads=[ykk, K_("gk")], writes=[ack])
                P.op("dve", lambda e, ht=ht, ac=ac: e.scalar_tensor_tensor(out=ac[:], in0=ht[:], scalar=float(ALPHA),
                                                                           in1=ac[:], op0=ALU.mult, op1=ALU.add),
                     reads=[hk, ack], writes=[ack])
            else:
                P.op("dve", lambda e, yk=yk, ac=ac, t=t, k=k: e.scalar_tensor_tensor(
                    out=ac[:], in0=yk[:], scalar=gk[:, t, k:k + 1], in1=ac[:], op0=ALU.mult, op1=ALU.add),
                    reads=[ykk, K_("gk"), ack], writes=[ack])
        layer_norm_tile(ac, ack, g2b, b2b, K_("g2b"), K_("b2b"))
        P.dma("act", lambda e, ac=ac, t=t: e.dma_start(out=T["hout"][t * 128:(t + 1) * 128, :], in_=ac[:]), reads=[ack])
    P.barrier()
    es3.close()


def declare_b_inputs(nc, NTOK, sfx=""):
    T = {}

    def din(name, shape, dt=F32):
        T[name] = nc.dram_tensor(name + sfx, list(shape), dt, kind="ExternalInput").ap()

    din("wout", [D, D])
    din("ln1g", [D])
    din("ln1b", [D])
    din("ln2g", [D])
    din("ln2b", [D])
    din("rw", [D, NE])
    din("rb", [NE])
    din("wgu", [NE, D, 2 * DFF])
    din("bgu", [NE, 2 * DFF])
    din("wd", [NE, DFF, D])
    din("bd", [NE, D])
    return T


def b_inputs(l, inputs):
    g = lambda k: np.ascontiguousarray(np.asarray(inputs[k][l]), dtype=np.float32)
    return dict(wout=g("w_out"), ln1g=g("ln1_g"), ln1b=g("ln1_b"), ln2g=g("ln2_g"), ln2b=g("ln2_b"), rw=g("router_w"),
                rb=g("router_b"), wgu=g("w_gate_up"), bgu=g("b_gate_up"), wd=g("w_down"), bd=g("b_down"))


def build_b(NTOK, C, stop_after=9):
    nc = bass.Bass("TRN2", target_bir_lowering=False)
    T = declare_b_inputs(nc, NTOK)
    declare_consts(nc, T)
    T["mixed"] = nc.dram_tensor("mixed", [NTOK, D], BF16, kind="ExternalInput").ap()
    T["hres"] = nc.dram_tensor("hres", [NTOK, D], F32, kind="ExternalInput").ap()
    T["hout"] = nc.dram_tensor("hout", [NTOK, D], F32, kind="ExternalOutput").ap()
    T["h1s"] = nc.dram_tensor("h1s", [NTOK, D], F32, kind="ExternalOutput").ap()
    T["xs"] = nc.dram_tensor("xs", [NE * C + 128, D], BF16, kind="Internal").ap()
    T["ys"] = nc.dram_tensor("ys", [NE * C + 128, D], BF16, kind="Internal").ap()
    with ExitStack() as es:
        P = Prog(nc, es)
        phase_b(nc, P, es, NTOK, C, T, "b_", stop_after)
        P.finish("sp")
        P.emit()
    return nc


S_FULL = 4096
NB = 4
NCORES = 8
_CACHE = {}


def _lam_init(l):
    return 0.8 - 0.6 * math.exp(-0.3 * l)


def build_fused(S, NTOK_B, C, HS=True):
    nc = bass.Bass("TRN2", target_bir_lowering=False)
    split = NTOK_B < S
    TC = {}
    declare_consts(nc, TC)
    TA = [declare_a_inputs(nc, S, True, sfx="_0")]
    for l in range(1, DEPTH):
        T = {}
        for name, shape, dt in (("pos", [S], I32), ("win", [D, NWIN], F32), ("wuq", [256, 1024], F32),
                                ("wukv", [128, 768], F32), ("bf", [4], F32), ("dlam", [128], F32), ("dg", [64], F32),
                                ("qg", [256], F32), ("kvg", [128], F32)):
            T[name] = nc.dram_tensor(f"{name}_{l}", shape, dt, kind="ExternalInput").ap()
        TA.append(T)
    TB = [declare_b_inputs(nc, NTOK_B, sfx=f"_{l}") for l in range(DEPTH)]
    rowidx = nc.dram_tensor("rowidx", [128, NTOK_B // 128], I32, kind="ExternalInput").ap() if split else None
    HS = HS and split
    if HS:
        rowidxg0 = nc.dram_tensor("rowidxg0", [128, NTOK_B // 128], I32, kind="ExternalInput").ap()
        rowidxg1 = nc.dram_tensor("rowidxg1", [128, NTOK_B // 128], I32, kind="ExternalInput").ap()
    hcur = None
    out = nc.dram_tensor("hout", [NTOK_B, D], F32, kind="ExternalOutput").ap()
    with ExitStack() as es:
        P = Prog(nc, es)
        for l in range(DEPTH):
            ta = dict(TA[l])
            ta.update(TC)
            ta["mixed"] = nc.dram_tensor(f"mixed_{l}", [S, D], BF16, kind="Internal").ap()
            if l == 0:
                ta["h0"] = nc.dram_tensor("h0", [S, D], F32, kind="Internal").ap()
                hres = ta["h0"]
            else:
                if split:
                    ta["hin_tile"] = hin_tile
                else:
                    ta["hin"] = hcur
                hres = hcur
            with ExitStack() as esa:
                phase_a(nc, P, esa, S, ta, l == 0, _lam_init(l), f"a{l}_", 9, 2 if HS else 4)
                P.barrier()
            tb = dict(TB[l])
            tb.update(TC)
            tb["mixed"] = ta["mixed"]
            if HS:
                CHM = 1024 if S >= 2048 else S // 2
                mixG = nc.dram_tensor(f"mixG_{l}", [2 * S, D], BF16, kind="Internal").ap()
                for k in range(S // CHM):
                    P.cc(lambda e, k=k, src=ta["mixed"], mixG=mixG: e.collective_compute(
                        "AllGather", ALU.bypass, replica_groups=[[2 * i, 2 * i + 1] for i in range(NCORES // 2)],
                        ins=[src[k * CHM:(k + 1) * CHM, :].opt()], outs=[mixG[k * 2 * CHM:(k + 1) * 2 * CHM, :].opt()]))
                P.barrier()
                tb["mixG"] = mixG
                tb["rowidxg0"] = rowidxg0
                tb["rowidxg1"] = rowidxg1
            tb["hres"] = hres
            if split:
                tb["rowidx"] = rowidx
                if l > 0:
                    tb["hres_local"] = hown_prev
            tb["h1s"] = nc.dram_tensor(f"h1s_{l}", [NTOK_B, D], F32, kind="Internal").ap()
            tb["xs"] = nc.dram_tensor(f"xs_{l}", [NE * C + 128, D], BF16, kind="Internal").ap()
            tb["ys"] = nc.dram_tensor(f"ys_{l}", [NE * C + 128, D], BF16, kind="Internal").ap()
            last = (l == DEPTH - 1)
            if last:
                tb["hout"] = out
            else:
                tb["hout"] = nc.dram_tensor(f"hown_{l}", [NTOK_B, D], F32, kind="Internal").ap()
            with ExitStack() as esb:
                phase_b(nc, P, esb, NTOK_B, C, tb, f"b{l}_")
                P.barrier()
            if not last:
                if split:
                    CH = 512 if NTOK_B >= 1024 else NTOK_B // 2
                    NCH = NTOK_B // CH
                    hfull = nc.dram_tensor(f"hfull_{l}", [S, D], F32, kind="Internal").ap()
                    src = tb["hout"]
                    hown_prev = src
                    for k in range(NCH):
                        P.cc(lambda e, src=src, hfull=hfull, k=k: e.collective_compute(
                            "AllGather", ALU.bypass, replica_groups=[[2 * i, 2 * i + 1] for i in range(NCORES // 2)],
                            ins=[src[k * CH:(k + 1) * CH, :].opt()], outs=[hfull[k * 2 * CH:(k + 1) * 2 * CH, :].opt()]))
                    P.barrier()
                    hcur = hfull

                    def hin_tile(t, hfull=hfull, CH=CH):
                        tok = t * 128
                        half, rem = tok // NTOK_B, tok % NTOK_B
                        k, r = rem // CH, rem % CH
                        row = k * 2 * CH + half * CH + r
                        return hfull[row:row + 128, :]
                else:
                    hcur = tb["hout"]
        P.finish("sp")
        P.emit()
    return nc


NTOK_B = 2048
C_B = 512


def kernel(**inputs):
    inputs = {k: np.asarray(v) for k, v in inputs.items()}
    x = inputs["x"].astype(np.float32, copy=False)
    pos = inputs["positions"].astype(np.int32, copy=False)
    consts = host_consts()
    cores = list(range(NCORES))
    if "f" not in _CACHE:
        _CACHE["f"] = build_fused(S_FULL, NTOK_B, C_B, True)
    nc = _CACHE["f"]
    shared = dict(consts)
    for l in range(DEPTH):
        for k_, v in b_inputs(l, inputs).items():
            shared[f"{k_}_{l}"] = v
    per_par = []
    for par in range(2):
        hp = (2 * par, 2 * par + 1, 2 * par, 2 * par + 1)
        d_ = {}
        for l in range(DEPTH):
            am = a_inputs(l, inputs, x[0], pos[0], l == 0, hp)
            for k_ in ("hin", "pos") + tuple(consts.keys()):
                am.pop(k_, None)
            for k_, v in am.items():
                d_[f"{k_}_{l}"] = v
        loc = np.arange(NTOK_B, dtype=np.int64)
        tok = par * NTOK_B + loc
        t2 = lambda a_: np.ascontiguousarray(a_.reshape(NTOK_B // 128, 128).T.astype(np.int32))
        CHM = 1024
        d_["rowidx"] = t2(tok)
        d_["rowidxg0"] = t2(((tok // CHM) * 2 + 0) * CHM + tok % CHM)
        d_["rowidxg1"] = t2(((tok // CHM) * 2 + 1) * CHM + tok % CHM)
        per_par.append(d_)
    maps = []
    for c in cores:
        b, par = c // 2, c % 2
        m = dict(shared)
        m.update(per_par[par])
        m["hin_0"] = np.ascontiguousarray(x[b])
        for l in range(DEPTH):
            m[f"pos_{l}"] = np.ascontiguousarray(pos[b])
        maps.append(m)
    res = run_bass_kernel_spmd(nc, maps, core_ids=cores).results
    return np.stack([np.concatenate([res[2 * b]["hout"], res[2 * b + 1]["hout"]], axis=0) for b in range(NB)]).astype(np.float32)
```
